# Optimizing a Trainium2 kernel written in Bass

```python
import jax, jax.numpy as jnp
from jax import lax
import numpy as np

D_MODEL = 2048
BATCH = 2
SEQ = 4096
DEPTH = 1

HEAD_DIM = 64
N_Q_HEADS = 16
N_KV_HEADS = 4
GQA_GROUP = N_Q_HEADS // N_KV_HEADS
ATTN_WIDTH = N_Q_HEADS * HEAD_DIM
KV_WIDTH = N_KV_HEADS * HEAD_DIM
WINDOW = 128
ATTN_BLOCK = 128
ROPE_THETA = 500000.0
ROT_DIM = HEAD_DIM // 4
POOL_WINDOWS = (2, 4, 8, 16)
N_POOL_GROUPS = len(POOL_WINDOWS)
POOL_WIDTH = D_MODEL // 2
POOL_GROUP_WIDTH = POOL_WIDTH // N_POOL_GROUPS
MIX_WIDTH = ATTN_WIDTH + POOL_WIDTH
IN_WIDTH = ATTN_WIDTH + 2 * KV_WIDTH + POOL_WIDTH
PEER_HEADS = 8
PEER_KEY_DIM = 256
PEER_HALF = PEER_KEY_DIM // 2
PEER_N_KEYS = 128
PEER_N_EXPERTS = PEER_N_KEYS * PEER_N_KEYS
PEER_TOPK = 16
PEER_CHUNK = 128
PLE_DIM = 256
RMS_EPS = 1e-6

kernel_name = "hymba_swa_pool_peer_block"


def rmsnorm(x, g):
    xf = x.astype(jnp.float32)
    y = xf * lax.rsqrt(jnp.mean(xf * xf, axis=-1, keepdims=True) + RMS_EPS)
    return (y * g.astype(jnp.float32)).astype(x.dtype)


def rope_tables(positions):
    inv_freq = 1.0 / (ROPE_THETA ** (jnp.arange(0, ROT_DIM, 2, dtype=jnp.float32) / ROT_DIM))
    ang = positions.astype(jnp.float32)[..., None] * inv_freq
    return jnp.cos(ang)[:, :, None, :], jnp.sin(ang)[:, :, None, :]


def apply_partial_rope(x, cos, sin):
    half = ROT_DIM // 2
    xr = x[..., :ROT_DIM].astype(jnp.float32)
    x1, x2 = xr[..., :half], xr[..., half:]
    rot = jnp.concatenate([x1 * cos - x2 * sin, x2 * cos + x1 * sin], axis=-1).astype(x.dtype)
    return jnp.concatenate([rot, x[..., ROT_DIM:]], axis=-1)


def sliding_window_attention(q, k, v, sinks):
    B, S = q.shape[0], q.shape[1]
    nb = S // ATTN_BLOCK
    qb = q.reshape(B, nb, ATTN_BLOCK, N_KV_HEADS, GQA_GROUP, HEAD_DIM)
    kb = k.reshape(B, nb, ATTN_BLOCK, N_KV_HEADS, HEAD_DIM)
    vb = v.reshape(B, nb, ATTN_BLOCK, N_KV_HEADS, HEAD_DIM)
    pad = ((0, 0), (1, 0), (0, 0), (0, 0), (0, 0))
    k_band = jnp.concatenate([jnp.pad(kb, pad)[:, :-1], kb], axis=2)
    v_band = jnp.concatenate([jnp.pad(vb, pad)[:, :-1], vb], axis=2)
    scale = HEAD_DIM ** -0.5
    s = jnp.einsum('bnqhgd,bnshd->bnhgqs', qb, k_band).astype(jnp.float32) * scale
    qi = jnp.arange(ATTN_BLOCK)[:, None] + ATTN_BLOCK
    kj = jnp.arange(2 * ATTN_BLOCK)[None, :]
    rel = qi - kj
    band = (rel >= 0) & (rel < WINDOW)
    has_prev = (jnp.arange(nb) > 0)[:, None, None] | (kj >= ATTN_BLOCK)[None]
    mask = band[None] & has_prev
    s = jnp.where(mask[None, :, None, None], s, jnp.finfo(jnp.float32).min)
    sink = sinks.astype(jnp.float32).reshape(N_KV_HEADS, GQA_GROUP)[None, None, :, :, None, None]
    sink = jnp.broadcast_to(sink, s.shape[:-1] + (1,))
    probs = jax.nn.softmax(jnp.concatenate([s, sink], axis=-1), axis=-1)[..., :-1]
    o = jnp.einsum('bnhgqs,bnshd->bnqhgd', probs.astype(v.dtype), v_band)
    return o.reshape(B, S, N_Q_HEADS * HEAD_DIM)


def multiscale_pool(u, w_pool, pool_scale):
    B, S = u.shape[0], u.shape[1]
    ug = u.reshape(B, S, N_POOL_GROUPS, POOL_GROUP_WIDTH)
    t = jnp.arange(S)
    outs = []
    for gi, w in enumerate(POOL_WINDOWS):
        xg = ug[:, :, gi, :].astype(jnp.float32)
        cs = jnp.cumsum(xg, axis=1)
        shifted = jnp.pad(cs, ((0, 0), (w, 0), (0, 0)))[:, :S]
        count = jnp.minimum(t + 1, w).astype(jnp.float32)[None, :, None]
        outs.append((cs - shifted) / count - xg)
    pooled = jnp.stack(outs, axis=2).astype(u.dtype)
    mixed = jnp.einsum('bsgc,gce->bsge', pooled, w_pool)
    return mixed.reshape(B, S, POOL_WIDTH) * pool_scale


def peer_ffn(xn, w_query, sub_keys, expert_u, expert_v):
    B, S, D = xn.shape
    q = (xn @ w_query).reshape(B, S, PEER_HEADS, PEER_KEY_DIM).astype(jnp.float32)
    sk = sub_keys.astype(jnp.float32)
    s1 = jnp.einsum('bshd,nd->bshn', q[..., :PEER_HALF], sk[0])
    s2 = jnp.einsum('bshd,nd->bshn', q[..., PEER_HALF:], sk[1])
    v1, i1 = lax.top_k(s1, PEER_TOPK)
    v2, i2 = lax.top_k(s2, PEER_TOPK)
    cand = (v1[..., :, None] + v2[..., None, :]).reshape(B, S, PEER_HEADS, PEER_TOPK * PEER_TOPK)
    cidx = (i1[..., :, None] * PEER_N_KEYS + i2[..., None, :]).reshape(B, S, PEER_HEADS, PEER_TOPK * PEER_TOPK)
    top, pos = lax.top_k(cand, PEER_TOPK)
    eidx = jnp.take_along_axis(cidx, pos, axis=-1)
    gates = jax.nn.softmax(top, axis=-1).astype(xn.dtype)
    T = B * S
    nc = T // PEER_CHUNK
    K = PEER_HEADS * PEER_TOPK
    xt = xn.reshape(nc, PEER_CHUNK, D)
    it = eidx.reshape(nc, PEER_CHUNK, K)
    gt = gates.reshape(nc, PEER_CHUNK, K)

    def expert_block(args):
        xc, ic, gc = args
        u = jnp.take(expert_u, ic, axis=0)
        h = jnp.einsum('cd,ckd->ck', xc, u)
        a = gc * jax.nn.gelu(h, approximate=False)
        v = jnp.take(expert_v, ic, axis=0)
        return jnp.einsum('ck,ckd->cd', a, v)

    out = lax.map(expert_block, (xt, it, gt))
    return out.reshape(B, S, D)


def setup_inputs(seed: int = 0) -> dict:
    key = jax.random.key(seed)
    ks = jax.random.split(key, 20)
    f32 = jnp.float32
    n = lambda k, shape, s: jax.random.normal(k, shape, f32) * s
    x = jax.random.normal(ks[0], (BATCH, SEQ, D_MODEL), f32)
    p = jax.random.normal(ks[1], (DEPTH, BATCH, SEQ, PLE_DIM), f32)
    offsets = jax.random.randint(ks[2], (BATCH, 1), 0, 1024, dtype=jnp.int32)
    positions = (offsets + jnp.arange(SEQ, dtype=jnp.int32)[None, :]).astype(jnp.int32)
    return {
        "x": x,
        "p": p,
        "positions": positions,
        "g_mix": 1.0 + n(ks[3], (DEPTH, D_MODEL), 0.05),
        "w_in": n(ks[4], (DEPTH, D_MODEL, IN_WIDTH), D_MODEL ** -0.5),
        "sinks": n(ks[5], (DEPTH, N_Q_HEADS), 0.5),
        "w_pool": n(ks[6], (DEPTH, N_POOL_GROUPS, POOL_GROUP_WIDTH, POOL_GROUP_WIDTH), POOL_GROUP_WIDTH ** -0.5),
        "pool_scale": 1.0 + n(ks[7], (DEPTH, POOL_WIDTH), 0.1),
        "w_out": n(ks[8], (DEPTH, MIX_WIDTH, D_MODEL), MIX_WIDTH ** -0.5),
        "g_ffn": 1.0 + n(ks[9], (DEPTH, D_MODEL), 0.05),
        "w_query": n(ks[10], (DEPTH, D_MODEL, PEER_HEADS * PEER_KEY_DIM), D_MODEL ** -0.5),
        "sub_keys": n(ks[11], (DEPTH, 2, PEER_N_KEYS, PEER_HALF), PEER_HALF ** -0.5),
        "expert_u": n(ks[12], (DEPTH, PEER_N_EXPERTS, D_MODEL), D_MODEL ** -0.5),
        "expert_v": n(ks[13], (DEPTH, PEER_N_EXPERTS, D_MODEL), 0.5),
        "g_ple": 1.0 + n(ks[14], (DEPTH, D_MODEL), 0.05),
        "w_ple_gate": n(ks[15], (DEPTH, D_MODEL, D_MODEL), D_MODEL ** -0.5),
        "w_ple_proj": n(ks[16], (DEPTH, PLE_DIM, D_MODEL), PLE_DIM ** -0.5),
        "g_final": 1.0 + n(ks[17], (D_MODEL,), 0.05),
    }


def reference(x, p, positions, g_mix, w_in, sinks, w_pool, pool_scale, w_out, g_ffn,
              w_query, sub_keys, expert_u, expert_v, g_ple, w_ple_gate, w_ple_proj, g_final):
    B, S = x.shape[0], x.shape[1]
    cos, sin = rope_tables(positions)
    h = x
    for i in range(DEPTH):
        hn = rmsnorm(h, g_mix[i])
        proj = hn @ w_in[i]
        q = proj[..., :ATTN_WIDTH].reshape(B, S, N_Q_HEADS, HEAD_DIM)
        k = proj[..., ATTN_WIDTH:ATTN_WIDTH + KV_WIDTH].reshape(B, S, N_KV_HEADS, HEAD_DIM)
        v = proj[..., ATTN_WIDTH + KV_WIDTH:ATTN_WIDTH + 2 * KV_WIDTH].reshape(B, S, N_KV_HEADS, HEAD_DIM)
        u = proj[..., ATTN_WIDTH + 2 * KV_WIDTH:]
        q = apply_partial_rope(q, cos, sin)
        k = apply_partial_rope(k, cos, sin)
        attn = sliding_window_attention(q, k, v, sinks[i])
        pool = multiscale_pool(u, w_pool[i], pool_scale[i])
        h = h + jnp.concatenate([attn, pool], axis=-1) @ w_out[i]
        h = h + peer_ffn(rmsnorm(h, g_ffn[i]), w_query[i], sub_keys[i], expert_u[i], expert_v[i])
        gate = jax.nn.sigmoid(rmsnorm(h, g_ple[i]) @ w_ple_gate[i])
        h = h + gate * (p[i] @ w_ple_proj[i])
    return rmsnorm(h, g_final)
```

```python
import math
import types
from contextlib import ExitStack

import numpy as np
import ml_dtypes

import concourse.bass as bass
import concourse.mybir as mybir
from concourse.bass_utils import run_bass_kernel_spmd

F32 = mybir.dt.float32
BF16 = mybir.dt.bfloat16
I32 = mybir.dt.int32
AF = mybir.ActivationFunctionType
OP = mybir.AluOpType
AX = mybir.AxisListType

D = 2048
NCH = 16
T = 512
HALO = 128
TH = T + HALO
NPASS = 2
NCORES = 8
NEXP_GROUPS = 32
SCALE = 64 ** -0.5
EPS = 1e-6
MARGIN = 2e-6
TWO_PI = 2.0 * math.pi

DEBUG = False


def _freeze(f, depth=0):
    if not isinstance(f, types.FunctionType) or depth > 4:
        return f
    cells = None
    if f.__closure__ is not None:
        cl = []
        for c in f.__closure__:
            try:
                cl.append(types.CellType(_freeze(c.cell_contents, depth + 1)))
            except ValueError:
                cl.append(c)
        cells = tuple(cl)
    dfl = f.__defaults__
    if dfl is not None:
        dfl = tuple(_freeze(d, depth + 1) for d in dfl)
    g = types.FunctionType(f.__code__, f.__globals__, f.__name__, dfl, cells)
    g.__kwdefaults__ = f.__kwdefaults__
    return g


class Tracker:
    ENGS = ["tensor", "vector", "scalar", "gpsimd", "sync"]

    def __init__(self, nc, es):
        self.nc = nc
        self.es = es
        self.prog = {e: [] for e in self.ENGS}
        self.sem = {e: es.enter_context(nc.semaphore("sem_" + e)) for e in self.ENGS}
        self.cnt = {e: 0 for e in self.ENGS}
        self.waited = {e: {} for e in self.ENGS}
        self.last_w = {}
        self.readers = {}
        self.dma_sems = {}
        self.dma_tokens = []

    def _wait(self, eng, tok):
        s, v, owner = tok
        if owner == eng and eng == "tensor":
            return
        if owner.startswith("dma:"):
            v = self.dma_sems[owner[4:]][1]
        w = self.waited[eng]
        if w.get(id(s), 0) >= v:
            return
        w[id(s)] = v
        self.prog[eng].append(lambda e, s=s, v=v: e.wait_ge(s, v))

    def _deps(self, eng, reads, writes):
        for k in reads:
            t = self.last_w.get(k)
            if t is not None:
                self._wait(eng, t)
        for k in writes:
            t = self.last_w.get(k)
            if t is not None:
                self._wait(eng, t)
            for t in self.readers.get(k, ()):
                self._wait(eng, t)

    def _record(self, tok, reads, writes):
        for k in reads:
            self.readers.setdefault(k, []).append(tok)
        for k in writes:
            self.last_w[k] = tok
            self.readers[k] = []

    def op(self, eng, reads, writes, fns):
        if not isinstance(fns, (list, tuple)):
            fns = [fns]
        fns = [_freeze(f) for f in fns]
        self._deps(eng, reads, writes)
        self.cnt[eng] += 1
        v = self.cnt[eng]
        s = self.sem[eng]
        for f in fns[:-1]:
            self.prog[eng].append(lambda e, f=f: f(e))
        last = fns[-1]
        self.prog[eng].append(lambda e, f=last, s=s: f(e).then_inc(s, 1))
        self._record((s, v, eng), reads, writes)

    def dma(self, queue, semname, reads, writes, fn):
        semname = queue + "_" + semname
        if semname not in self.dma_sems:
            self.dma_sems[semname] = [self.es.enter_context(self.nc.semaphore("dsem_" + semname)), 0]
        ent = self.dma_sems[semname]
        fn = _freeze(fn)
        self._deps(queue, reads, writes)
        ent[1] += 16
        s, v = ent[0], ent[1]
        self.prog[queue].append(lambda e, f=fn, s=s: f(e).then_inc(s, 16))
        tok = (s, v, "dma:" + semname)
        self._record(tok, reads, writes)
        self.dma_tokens.append(tok)

    def barrier(self):
        toks = [(self.sem[e], self.cnt[e], e) for e in self.ENGS if self.cnt[e] > 0]
        toks += self.dma_tokens
        self.dma_tokens = []
        for e in self.ENGS:
            for t in toks:
                self._wait(e, t)
        self.last_w = {}
        self.readers = {}
        self.flush()

    def flush(self):
        for eng in self.ENGS:
            prog = self.prog[eng]
            if not prog:
                continue
            self.prog[eng] = []

            def body(e, prog=prog):
                for f in prog:
                    f(e)
            getattr(self.block, eng)(body)

    def emit(self, block):
        tr = self

        @block.tensor
        def _(e):
            for f in tr.prog["tensor"]:
                f(e)

        @block.vector
        def _(e):
            for f in tr.prog["vector"]:
                f(e)

        @block.scalar
        def _(e):
            for f in tr.prog["scalar"]:
                f(e)

        @block.gpsimd
        def _(e):
            for f in tr.prog["gpsimd"]:
                f(e)

        @block.sync
        def _(e):
            for f in tr.prog["sync"]:
                f(e)


def build_program():
    nc = bass.Bass("TRN2", target_bir_lowering=False)

    def din(name, shape, dt=F32):
        return nc.dram_tensor(name, list(shape), dt, kind="ExternalInput").ap()

    xT_d = din("xT", [NPASS, D, TH])
    pT_d = din("pT", [NPASS, 256, T])
    pos_d = din("posb", [NPASS, 128, TH], I32)
    maskp_d = din("maskp", [NPASS, 128, 256])
    maskg_d = din("maskg", [128, 256])
    icnt_d = din("icnt", [NPASS, 128, 4, 16])
    wqku_d = din("wqku", [D, 2560])
    wv_d = din("wv", [D, 256])
    wpool_d = din("wpool", [4, 256, 256])
    pscale_d = din("pscale", [128, 8])
    gvec_d = din("gvec", [128, 4, NCH])
    sinks_d = din("sinksb", [128, 16])
    wout_d = din("wout", [D, D])
    wquery_d = din("wquery", [D, D])
    skT_d = din("skT", [2, 128, 128])
    UT_d = din("UT", [D, 16384])
    V_d = din("Vx", [16384, D])
    wgate_d = din("wgate", [D, D])
    wproj_d = din("wproj", [256, D])
    ident_d = din("ident", [128, 128])
    perm_d = din("perm", [128, 128])
    ropec_d = din("ropec", [128, 2])
    out_d = nc.dram_tensor("outT", [NPASS, D, T], F32, kind="ExternalOutput").ap()
    dbg_d = None
    if DEBUG:
        dbg_d = nc.dram_tensor("dbg", [NPASS, 3, D, T], F32, kind="ExternalOutput").ap()

    with ExitStack() as es:
        tr = Tracker(nc, es)

        uniq = [0]

        def sb(stack, name, shape, dt):
            uniq[0] += 1
            return stack.enter_context(nc.sbuf_tensor(f"sb{uniq[0]}_{name}", list(shape), dt))

        ident = sb(es, "ident", [128, 128], BF16)
        permf = sb(es, "permf", [128, 128], F32)
        onesf = sb(es, "onesf", [128, 128], F32)
        epsT = sb(es, "epsT", [128, 1], F32)
        maskg = sb(es, "maskg", [128, 256], BF16)
        maskp = sb(es, "maskp", [128, 256], BF16)
        gvec = sb(es, "gvec", [128, 4, NCH], F32)
        sinks = sb(es, "sinks", [128, 16], F32)
        pscale = sb(es, "pscale", [128, 8], F32)
        ropec = sb(es, "ropec", [128, 2], F32)
        skT = sb(es, "skT", [128, 2, 128], BF16)
        hT = sb(es, "hT", [128, NCH, T], F32)
        xn = sb(es, "xn", [128, NCH, T], BF16)
        s_all = sb(es, "s_all", [128, 4, 8, 2, 128], F32)
        tcm = sb(es, "tcm", [128, 4, 8], F32)
        dg = sb(es, "dg", [128, 4, 8, 128], BF16)

        psall = es.enter_context(nc.psum_tensor("psall", [128, 7 * 512], F32))
        banks = [psall[:, i * 512:(i + 1) * 512] for i in range(7)]
        ptb = es.enter_context(nc.psum_tensor("ptb", [128, 1024], BF16))

        block = es.enter_context(nc.Block())
        tr.block = block

        dumps = {}

        def dump(name, ap, shape, keys, ps_):
            if not DEBUG or ps_ != 0:
                return
            dd_ = nc.dram_tensor("dbg_" + name, list(shape), F32, kind="ExternalOutput").ap()
            dumps[name] = dd_
            tr.dma("gpsimd", "dbgd", keys, [], lambda e: e.dma_start(out=dd_, in_=ap))

        def act_copy(out, in_):
            return lambda e: e.activation(out=out, in_=in_, func=AF.Copy)

        tr.dma("gpsimd", "c", [], ["ident"], lambda e: e.dma_start(out=ident[:], in_=ident_d[:, :]))
        tr.dma("sync", "c", [], ["permf"], lambda e: e.dma_start(out=permf[:], in_=perm_d[:, :]))
        tr.dma("gpsimd", "c", [], ["maskg"], lambda e: e.dma_start(out=maskg[:], in_=maskg_d[:, :]))
        tr.dma("sync", "c", [], ["gvec"], lambda e: e.dma_start(out=gvec[:], in_=gvec_d[:, :, :]))
        tr.dma("sync", "c", [], ["sinks"], lambda e: e.dma_start(out=sinks[:], in_=sinks_d[:, :]))
        tr.dma("sync", "c", [], ["pscale"], lambda e: e.dma_start(out=pscale[:], in_=pscale_d[:, :]))
        tr.dma("sync", "c", [], ["ropec"], lambda e: e.dma_start(out=ropec[:], in_=ropec_d[:, :]))
        tr.dma("gpsimd", "c", [], ["skT"],
               lambda e: e.dma_start(out=skT[:], in_=skT_d.rearrange("a k n -> k a n")))
        tr.op("vector", [], ["onesf"], lambda e: e.memset(onesf[:], 1.0))
        tr.op("vector", [], ["epsT"], lambda e: e.memset(epsT[:], EPS))

        def rmsnorm(stack_tag, src, srckey, n, gi, dst, dstkey, bank_a, sq, rs, col0=0):
            pa = banks[bank_a]
            for c in range(NCH):
                sqc = sq[c % 2]
                tr.op("scalar", [(srckey, c)], [("sq", id(sqc))],
                      lambda e, c=c, sqc=sqc: e.activation(out=sqc[:, 0:n], in_=src[:, c, 0:n], func=AF.Square))
                tr.op("tensor", [("sq", id(sqc)), "onesf"], [("bank", bank_a)],
                      lambda e, c=c, sqc=sqc: e.matmul(pa[:, 0:n], lhsT=onesf[:], rhs=sqc[:, 0:n],
                                                       start=(c == 0), stop=(c == NCH - 1)))
            tr.op("scalar", [("bank", bank_a), "epsT"], [("rs", id(rs))],
                  lambda e: e.activation(out=rs[:, 0:n], in_=pa[:, 0:n], func=AF.Sqrt, bias=epsT[:], scale=1.0 / D))
            tr.op("vector", [("rs", id(rs))], [("rs", id(rs))],
                  lambda e: e.reciprocal(out=rs[:, 0:n], in_=rs[:, 0:n]))
            for c in range(NCH):
                tr.op("vector", [(srckey, c), ("rs", id(rs)), "gvec"], [(dstkey, c)],
                      lambda e, c=c: e.scalar_tensor_tensor(out=dst[:, c, col0:col0 + n], in0=src[:, c, 0:n],
                                                            scalar=gvec[:, gi, c:c + 1], in1=rs[:, 0:n],
                                                            op0=OP.mult, op1=OP.mult))

        wslot_ctr = [0]

        def load_w(wbuf, fn_dma_list):
            s = wslot_ctr[0] % len(wbuf)
            wslot_ctr[0] += 1
            for fn in fn_dma_list:
                tr.dma("gpsimd", f"w{s}", [], [("wbuf", s)], lambda e, fn=fn, s=s: fn(e, wbuf[s]))
            return s

        for ps_ in range(NPASS):
            with ExitStack() as p1:
                wbuf = [sb(p1, f"wbuf{i}", [128, NCH + 8, 512], BF16) for i in range(2)]
                qT = sb(p1, "qT", [128, 8, T], BF16)
                kT = sb(p1, "kT", [128, 4, TH], BF16)
                Vsb = sb(p1, "Vsb", [128, 5, 256], BF16)
                uT = sb(p1, "uT", [128, 8, TH], F32)
                icnt = sb(p1, "icnt", [128, 4, 16], F32)
                wpool = sb(p1, "wpool", [128, 4, 2, 256], BF16)

                tr.dma("gpsimd", "c2", [], ["maskp"], lambda e: e.dma_start(out=maskp[:], in_=maskp_d[ps_, :, :]))
                tr.dma("sync", "c2", [], ["icnt"], lambda e: e.dma_start(out=icnt[:], in_=icnt_d[ps_, :, :, :]))
                tr.dma("gpsimd", "c2", [], ["wpool"],
                       lambda e: e.dma_start(out=wpool[:], in_=wpool_d.rearrange("g (cc p) n -> p g cc n", p=128)))
                for q4 in range(4):
                    tr.dma("sync", "x", [], [("hT", c) for c in range(4 * q4, 4 * q4 + 4)],
                           lambda e, q4=q4: e.dma_start(
                               out=hT[:, 4 * q4:4 * q4 + 4, :],
                               in_=xT_d[ps_, 512 * q4:512 * q4 + 512, HALO:TH].rearrange("(c p) t -> p c t", p=128)))

                with ExitStack() as a1:
                    xh = xn[:].rearrange("p c t -> p (c t)").bitcast(F32)[:, 0:NCH * HALO].rearrange(
                        "p (c t) -> p c t", c=NCH)
                    hn = s_all[:].rearrange("p a b c d -> p (a b c d)").bitcast(BF16)[:, 0:NCH * TH].rearrange(
                        "p (c t) -> p c t", c=NCH)
                    sq = [sb(a1, f"sq{i}", [128, T], F32) for i in range(2)]
                    rs = sb(a1, "rs", [128, T], F32)
                    rs2 = sb(a1, "rs2", [128, HALO], F32)
                    qraw = [sb(a1, f"qraw{i}", [128, TH], F32) for i in range(2)]
                    rt1 = [sb(a1, f"rt1_{i}", [128, TH], F32) for i in range(2)]
                    rt2 = [sb(a1, f"rt2_{i}", [128, TH], F32) for i in range(2)]
                    Ct = sb(a1, "Ct", [128, TH], F32)
                    St = sb(a1, "St", [128, TH], F32)
                    posi = qraw[1][:].bitcast(I32)
                    K_POSI = [("qraw", 1, 0), ("qraw", 1, 1)]
                    ang, ra, rb, ki = rt1[0], rt1[1], rt2[0], posi
                    K_ANG, K_RA = ("rt1", 0), ("rt1", 1)
                    K_RB = [("rt2", 0, 0), ("rt2", 0, 1)]

                    tr.dma("sync", "x", [], [("xh", c) for c in range(NCH)],
                           lambda e: e.dma_start(out=xh[:, :, :], in_=xT_d[ps_, :, 0:HALO].rearrange("(c p) t -> p c t", p=128)))
                    tr.dma("sync", "c2", [], K_POSI, lambda e: e.dma_start(out=posi, in_=pos_d[ps_, :, :]))

                    tr.op("vector", K_POSI, [K_ANG], lambda e: e.tensor_copy(out=ang[:], in_=posi))
                    tr.op("vector", [K_ANG, "ropec"], [K_ANG],
                          lambda e: e.tensor_scalar(out=ang[:], in0=ang[:], scalar1=ropec[:, 0:1], scalar2=None, op0=OP.mult))

                    def sin_of(shift, dst, sgn):
                        tr.op("vector", [K_ANG], [K_RA],
                              lambda e: e.tensor_scalar(out=ra[:], in0=ang[:], scalar1=1.0 / TWO_PI,
                                                        scalar2=shift / TWO_PI, op0=OP.mult, op1=OP.add))
                        tr.op("vector", [K_RA], K_POSI, lambda e: e.tensor_copy(out=ki, in_=ra[:]))
                        tr.op("vector", K_POSI, [K_RA], lambda e: e.tensor_copy(out=ra[:], in_=ki))
                        tr.op("vector", [K_RA], [K_RA],
                              lambda e: e.tensor_scalar(out=ra[:], in0=ra[:], scalar1=-TWO_PI, scalar2=shift,
                                                        op0=OP.mult, op1=OP.add))
                        tr.op("vector", [K_RA, K_ANG], K_RB,
                              lambda e: e.tensor_tensor(out=rb[:], in0=ra[:], in1=ang[:], op=OP.add))
                        tr.op("vector", K_RB, [K_RA],
                              lambda e: e.tensor_scalar(out=ra[:], in0=rb[:], scalar1=math.pi, scalar2=-TWO_PI,
                                                        op0=OP.is_gt, op1=OP.mult))
                        tr.op("vector", [K_RA] + K_RB, K_RB,
                              lambda e: e.tensor_tensor(out=rb[:], in0=rb[:], in1=ra[:], op=OP.add))
                        tr.op("vector", K_RB, [K_RA],
                              lambda e: e.tensor_scalar(out=ra[:], in0=rb[:], scalar1=-math.pi, scalar2=TWO_PI,
                                                        op0=OP.is_lt, op1=OP.mult))
                        tr.op("vector", [K_RA] + K_RB, K_RB,
                              lambda e: e.tensor_tensor(out=rb[:], in0=rb[:], in1=ra[:], op=OP.add))
                        tr.op("vector", K_RB, K_RB,
                              lambda e: e.tensor_scalar(out=rb[:], in0=rb[:], scalar1=3.1415925, scalar2=-3.1415925,
                                                        op0=OP.min, op1=OP.max))
                        tr.op("scalar", K_RB, [dst[1]], lambda e: e.activation(out=dst[0][:], in_=rb[:], func=AF.Sin))
                        if sgn:
                            tr.op("vector", [dst[1], "ropec"], [dst[1]],
                                  lambda e: e.tensor_scalar(out=dst[0][:], in0=dst[0][:], scalar1=ropec[:, 1:2],
                                                            scalar2=None, op0=OP.mult))

                    sin_of(0.0, (St, "St"), True)
                    sin_of(math.pi / 2, (Ct, "Ct"), False)

                    rmsnorm("a", hT, "hT", T, 0, hn, "hn_o", 0, sq, rs, col0=HALO)
                    rmsnorm("b", xh, "xh", HALO, 0, hn, "hn_h", 1, sq, rs2, col0=0)
                    hn_keys = [("hn_o", c) for c in range(NCH)] + [("hn_h", c) for c in range(NCH)]

                    sv = load_w(wbuf, [lambda e, wb: e.dma_start(
                        out=wb[:, 0:NCH, 0:256], in_=wv_d.rearrange("(c p) n -> p c n", p=128))])
                    nxt = load_w(wbuf, [lambda e, wb: e.dma_start(
                        out=wb[:, 0:NCH, :], in_=wqku_d[:, 0:512].rearrange("(c p) n -> p c n", p=128))])
                    for tt in range(5):
                        bk = 2 + tt % 2
                        tr.op("tensor", hn_keys + [("wbuf", sv)], [("bank", bk)],
                              [lambda e, c=c, tt=tt, bk=bk: e.matmul(banks[bk][:, 0:256],
                                                                      lhsT=hn[:, c, tt * 128:(tt + 1) * 128],
                                                                      rhs=wbuf[sv][:, c, 0:256],
                                                                      start=(c == 0), stop=(c == NCH - 1))
                               for c in range(NCH)])
                        tr.op("scalar", [("bank", bk)], [("Vsb", tt)],
                              act_copy(Vsb[:, tt, :], banks[bk][:, 0:256]))

                    ev = 0
                    for cg in range(5):
                        cur = nxt
                        if cg < 4:
                            nxt = load_w(wbuf, [lambda e, wb, cg=cg: e.dma_start(
                                out=wb[:, 0:NCH, :],
                                in_=wqku_d[:, 512 * (cg + 1):512 * (cg + 2)].rearrange("(c p) n -> p c n", p=128))])
                        for oc in range(4):
                            kind = "q" if cg < 2 else ("k" if cg == 2 else "u")
                            ba, bb = (2, 3) if ev % 2 == 0 else (4, 5)
                            ev += 1
                            tr.op("tensor", hn_keys + [("wbuf", cur)], [("bank", ba)],
                                  [lambda e, c=c, oc=oc, ba=ba, cur=cur: e.matmul(
                                      banks[ba][:, :], lhsT=wbuf[cur][:, c, oc * 128:(oc + 1) * 128],
                                      rhs=hn[:, c, HALO:TH], start=(c == 0), stop=(c == NCH - 1))
                                   for c in range(NCH)])
                            if kind != "q":
                                tr.op("tensor", hn_keys + [("wbuf", cur)], [("bank", bb)],
                                      [lambda e, c=c, oc=oc, bb=bb, cur=cur: e.matmul(
                                          banks[bb][:, 0:HALO], lhsT=wbuf[cur][:, c, oc * 128:(oc + 1) * 128],
                                          rhs=hn[:, c, 0:HALO], start=(c == 0), stop=(c == NCH - 1))
                                       for c in range(NCH)])
                            if kind == "u":
                                ch = (cg - 3) * 4 + oc
                                tr.op("scalar", [("bank", ba)], [("uT", ch, 1)],
                                      act_copy(uT[:, ch, HALO:TH], banks[ba][:, :]))
                                tr.op("scalar", [("bank", bb)], [("uT", ch, 0)],
                                      act_copy(uT[:, ch, 0:HALO], banks[bb][:, 0:HALO]))
                                continue
                            qi = ev % 2
                            qr = qraw[qi]
                            lo = HALO if kind == "q" else 0
                            tr.op("scalar", [("bank", ba)], [("qraw", qi, 1)], act_copy(qr[:, HALO:TH], banks[ba][:, :]))
                            if kind == "k":
                                tr.op("scalar", [("bank", bb)], [("qraw", qi, 0)],
                                      act_copy(qr[:, 0:HALO], banks[bb][:, 0:HALO]))
                            tr.op("tensor", [("qraw", qi, 1), "permf"], [("bank", ba)],
                                  lambda e, qr=qr, ba=ba: e.matmul(banks[ba][:, :], lhsT=permf[:], rhs=qr[:, HALO:TH],
                                                                   start=True, stop=True))
                            if kind == "k":
                                tr.op("tensor", [("qraw", qi, 0), "permf"], [("bank", bb)],
                                      lambda e, qr=qr, bb=bb: e.matmul(banks[bb][:, 0:HALO], lhsT=permf[:],
                                                                       rhs=qr[:, 0:HALO], start=True, stop=True))
                            t1 = rt1[qi]
                            t2 = rt2[qi]
                            tr.op("gpsimd", [("qraw", qi, 1), ("qraw", qi, 0), "Ct"], [("rt1", qi)],
                                  lambda e, qr=qr, t1=t1, lo=lo: e.tensor_tensor(out=t1[:, lo:TH], in0=qr[:, lo:TH],
                                                                                in1=Ct[:, lo:TH], op=OP.mult))
                            tr.op("vector", [("bank", ba), "St"], [("rt2", qi, 1)],
                                  lambda e, t2=t2, ba=ba: e.tensor_tensor(out=t2[:, HALO:TH], in0=banks[ba][:, :],
                                                                          in1=St[:, HALO:TH], op=OP.mult))
                            if kind == "k":
                                tr.op("vector", [("bank", bb), "St"], [("rt2", qi, 0)],
                                      lambda e, t2=t2, bb=bb: e.tensor_tensor(out=t2[:, 0:HALO], in0=banks[bb][:, 0:HALO],
                                                                              in1=St[:, 0:HALO], op=OP.mult))
                            if kind == "q":
                                ch = cg * 4 + oc
                                tr.op("gpsimd", [("rt1", qi), ("rt2", qi, 1)], [("qT", ch)],
                                      lambda e, t1=t1, t2=t2, ch=ch: e.tensor_tensor(out=qT[:, ch, :], in0=t1[:, HALO:TH],
                                                                                    in1=t2[:, HALO:TH], op=OP.add))
                            else:
                                tr.op("gpsimd", [("rt1", qi), ("rt2", qi, 1), ("rt2", qi, 0)], [("kT", oc)],
                                      lambda e, t1=t1, t2=t2, oc=oc: e.tensor_tensor(out=kT[:, oc, :], in0=t1[:, :],
                                                                                    in1=t2[:, :], op=OP.add))
                    dump("hn", hn, [128, NCH, TH], hn_keys, ps_)
                    dump("qT", qT[:], [128, 8, T], [("qT", c) for c in range(8)], ps_)
                    dump("kT", kT[:], [128, 4, TH], [("kT", c) for c in range(4)], ps_)
                    dump("Vsb", Vsb[:], [128, 5, 256], [("Vsb", c) for c in range(5)], ps_)
                    dump("uT", uT[:], [128, 8, TH], [("uT", c, i) for c in range(8) for i in range(2)], ps_)
                    dump("Ct", Ct[:], [128, TH], ["Ct"], ps_)
                    dump("St", St[:], [128, TH], ["St"], ps_)
                    tr.barrier()

                with ExitStack() as a2:
                    Pf = [sb(a2, f"Pf{i}", [128, 4, 256], F32) for i in range(2)]
                    Pn = [sb(a2, f"Pn{i}", [128, 4, 256], BF16) for i in range(2)]
                    PTs = [sb(a2, f"PTs{i}", [128, 1024], BF16) for i in range(2)]
                    sm = [sb(a2, f"sm{i}", [128, 8, 4], F32) for i in range(2)]
                    nsinks = sb(a2, "nsinks", [128, 16], F32)
                    attnT = s_all[:].rearrange("p a b c d -> p (a b c d)").bitcast(BF16)[0:64, 0:16 * T].rearrange(
                        "p (h t) -> p h t", h=16)
                    pooled = xn[:, 0:8, :]
                    mixp = xn[:, 8:16, :]
                    pa_ = sb(a2, "pa_", [128, TH], F32)
                    pb_ = sb(a2, "pb_", [128, TH], F32)

                    def attn_front(it):
                        qb, g = it // 4, it % 4
                        msk, mkey = (maskp, "maskp") if qb == 0 else (maskg, "maskg")
                        pi = it % 2
                        b0, b1 = (0, 1) if pi == 0 else (2, 3)
                        smt = sm[pi]
                        fns = []
                        for hh in range(4):
                            hd = 4 * g + hh
                            bp = (hd % 2) * 64
                            bk = b0 if hh < 2 else b1
                            co = (hh % 2) * 256
                            fns.append(lambda e, hd=hd, bp=bp, bk=bk, co=co, qb=qb, g=g: e.matmul(
                                banks[bk][:, co:co + 256], lhsT=qT[bp:bp + 64, hd // 2, qb * 128:(qb + 1) * 128],
                                rhs=kT[bp:bp + 64, g, qb * 128:qb * 128 + 256], start=True, stop=False))
                            fns.append(lambda e, bk=bk, co=co, msk=msk: e.matmul(
                                banks[bk][:, co:co + 256], lhsT=ident[:], rhs=msk[:], start=False, stop=True))
                        tr.op("tensor", [("qT", c) for c in (2 * g, 2 * g + 1)] + [("kT", g), "ident", mkey],
                              [("bank", b0), ("bank", b1)], fns)
                        for half, bk in ((0, b0), (1, b1)):
                            tr.op("vector", [("bank", bk)], [("sm", pi, "mx", half)],
                                  lambda e, bk=bk, half=half, smt=smt: e.tensor_reduce(
                                      out=smt[:, 0, 2 * half:2 * half + 2],
                                      in_=banks[bk][:, :].rearrange("p (h k) -> p h k", h=2), axis=AX.X, op=OP.max))
                        tr.op("vector", [("sm", pi, "mx", 0), ("sm", pi, "mx", 1), "sinks"], [("sm", pi, "m2")],
                              lambda e, smt=smt, g=g: e.scalar_tensor_tensor(
                                  out=smt[:, 2, :], in0=smt[:, 0, :], scalar=-SCALE, in1=nsinks[:, 4 * g:4 * g + 4],
                                  op0=OP.mult, op1=OP.min))
                        tr.op("vector", [("sm", pi, "m2"), "sinks"], [("sm", pi, "m")],
                              lambda e, smt=smt, g=g: e.tensor_tensor(out=smt[:, 3, :], in0=smt[:, 2, :],
                                                                      in1=sinks[:, 4 * g:4 * g + 4], op=OP.add))
                        pf = Pf[pi]
                        fns = []
                        for hh in range(4):
                            bk = b0 if hh < 2 else b1
                            co = (hh % 2) * 256
                            fns.append(lambda e, hh=hh, bk=bk, co=co, pf=pf, smt=smt: e.activation(
                                out=pf[:, hh, :], in_=banks[bk][:, co:co + 256], func=AF.Exp,
                                bias=smt[:, 2, hh:hh + 1], scale=SCALE, accum_out=smt[:, 4, hh:hh + 1]))
                        fns.append(lambda e, smt=smt: e.activation(out=smt[:, 5, :], in_=smt[:, 3, :], func=AF.Exp))
                        tr.op("scalar", [("bank", b0), ("bank", b1), ("sm", pi, "m"), ("sm", pi, "m2")],
                              [("Pf", pi), ("sm", pi, "z")], fns)

                    def attn_back(it):
                        qb, g = it // 4, it % 4
                        pi = it % 2
                        bo = 4 + pi
                        smt = sm[pi]
                        pf = Pf[pi]
                        pn = Pn[pi]
                        tr.op("vector", [("sm", pi, "z")], [("sm", pi, "zz")],
                              lambda e, smt=smt: e.tensor_tensor(out=smt[:, 6, :], in0=smt[:, 4, :], in1=smt[:, 5, :],
                                                                 op=OP.add))
                        tr.op("vector", [("sm", pi, "zz")], [("sm", pi, "rz")],
                              lambda e, smt=smt: e.reciprocal(out=smt[:, 7, :], in_=smt[:, 6, :]))
                        tr.op("vector", [("Pf", pi), ("sm", pi, "rz")], [("Pn", pi)],
                              lambda e, smt=smt, pf=pf, pn=pn: e.tensor_tensor(
                                  out=pn[:], in0=pf[:], in1=smt[:, 7, :].unsqueeze(2).to_broadcast([128, 4, 256]),
                                  op=OP.mult))
                        fns = []
                        for kb in range(2):
                            for hh in range(4):
                                o = (kb * 4 + hh) * 128
                                fns.append(lambda e, kb=kb, hh=hh, o=o, pn=pn: e.transpose(
                                    out=ptb[:, o:o + 128], in_=pn[:, hh, kb * 128:(kb + 1) * 128], identity=ident[:]))
                        tr.op("tensor", [("Pn", pi), "ident"], ["ptb"], fns)
                        pts = PTs[pi]
                        tr.op("scalar", ["ptb"], [("PTs", pi)], act_copy(pts[:], ptb[:]))
                        tr.op("tensor", [("PTs", pi), ("Vsb", qb), ("Vsb", qb + 1)], [("bank", bo)],
                              [lambda e, kb=kb, pts=pts, bo=bo, qb=qb, g=g: e.matmul(
                                  banks[bo][0:64, :], lhsT=Vsb[:, qb + kb, g * 64:(g + 1) * 64],
                                  rhs=pts[:, kb * 512:(kb + 1) * 512], start=(kb == 0), stop=(kb == 1))
                               for kb in range(2)])
                        tr.op("vector", [("bank", bo)], [("attnT", 4 * g + hh, qb) for hh in range(4)],
                              lambda e, bo=bo, qb=qb, g=g: e.tensor_copy(
                                  out=attnT[:, 4 * g:4 * g + 4, qb * 128:(qb + 1) * 128],
                                  in_=banks[bo][0:64, :].rearrange("p (h q) -> p h q", h=4)))

                    tr.op("vector", ["sinks"], ["nsinks"],
                          lambda e: e.tensor_scalar(out=nsinks[:], in0=sinks[:], scalar1=-1.0, scalar2=None, op0=OP.mult))
                    attn_front(0)
                    for it in range(16):
                        if it + 1 < 16:
                            attn_front(it + 1)
                        attn_back(it)

                    for ch in range(8):
                        gi = ch // 2
                        w = 2 ** (gi + 1)
                        src = None
                        bufs = [pa_, pb_]
                        names = ["pa_", "pb_"]
                        cur_ap, cur_key = None, None
                        sft = 1
                        lvl = 0
                        while sft < w:
                            dst = bufs[lvl % 2]
                            dk = names[lvl % 2]
                            lo_ = HALO - 17 + 2 * sft
                            if lvl == 0:
                                tr.op("gpsimd", [("uT", ch, 0), ("uT", ch, 1)], [dk],
                                      lambda e, dst=dst, sft=sft, ch=ch, lo_=lo_: e.tensor_tensor(
                                          out=dst[:, lo_:TH], in0=uT[:, ch, lo_:TH], in1=uT[:, ch, lo_ - sft:TH - sft], op=OP.add))
                            else:
                                srcb = bufs[(lvl - 1) % 2]
                                sk_ = names[(lvl - 1) % 2]
                                tr.op("gpsimd", [sk_], [dk],
                                      lambda e, dst=dst, srcb=srcb, sft=sft, lo_=lo_: e.tensor_tensor(
                                          out=dst[:, lo_:TH], in0=srcb[:, lo_:TH], in1=srcb[:, lo_ - sft:TH - sft], op=OP.add))
                            cur_ap, cur_key = dst, dk
                            sft *= 2
                            lvl += 1
                        tr.op("gpsimd", [cur_key, "icnt"], [cur_key],
                              lambda e, cur_ap=cur_ap, gi=gi: e.tensor_tensor(
                                  out=cur_ap[:, HALO:HALO + 16], in0=cur_ap[:, HALO:HALO + 16], in1=icnt[:, gi, :], op=OP.mult))
                        tr.op("gpsimd", [cur_key], [cur_key],
                              lambda e, cur_ap=cur_ap, w=w: e.tensor_scalar(
                                  out=cur_ap[:, HALO + 16:TH], in0=cur_ap[:, HALO + 16:TH], scalar1=1.0 / w, scalar2=None,
                                  op0=OP.mult))
                        tr.op("gpsimd", [cur_key, ("uT", ch, 1)], [("pooled", ch)],
                              lambda e, cur_ap=cur_ap, ch=ch: e.tensor_tensor(
                                  out=pooled[:, ch, :], in0=cur_ap[:, HALO:TH], in1=uT[:, ch, HALO:TH], op=OP.subtract))
                    for ch in range(8):
                        gi, ec = ch // 2, ch % 2
                        bk = ch % 2
                        tr.op("tensor", [("pooled", 2 * gi), ("pooled", 2 * gi + 1), "wpool"], [("bank", bk)],
                              [lambda e, cc=cc, gi=gi, ec=ec, bk=bk: e.matmul(
                                  banks[bk][:, :], lhsT=wpool[:, gi, cc, ec * 128:(ec + 1) * 128],
                                  rhs=pooled[:, 2 * gi + cc, :], start=(cc == 0), stop=(cc == 1)) for cc in range(2)])
                        tr.op("scalar", [("bank", bk), "pscale"], [("mixp", ch)],
                              lambda e, ch=ch, bk=bk: e.activation(out=mixp[:, ch, :], in_=banks[bk][:, :], func=AF.Copy,
                                                                   scale=pscale[:, ch:ch + 1]))

                    dump("attnT", attnT, [64, 16, T], [("attnT", hd, qb) for hd in range(16) for qb in range(4)], ps_)
                    dump("pooled", pooled, [128, 8, T], [("pooled", c) for c in range(8)], ps_)
                    dump("mixp", mixp, [128, 8, T], [("mixp", c) for c in range(8)], ps_)

                    def ld_wout(cg):
                        return load_w(wbuf, [
                            lambda e, wb, cg=cg: e.dma_start(
                                out=wb[0:64, 0:16, :],
                                in_=wout_d[0:1024, 512 * cg:512 * cg + 512].rearrange("(h p) n -> p h n", p=64)),
                            lambda e, wb, cg=cg: e.dma_start(
                                out=wb[:, 16:24, :],
                                in_=wout_d[1024:2048, 512 * cg:512 * cg + 512].rearrange("(c p) n -> p c n", p=128))])

                    nxt = ld_wout(0)
                    for cg in range(4):
                        cur = nxt
                        if cg < 3:
                            nxt = ld_wout(cg + 1)
                        for oc in range(4):
                            dc = cg * 4 + oc
                            bk = 2 + dc % 2
                            fns = [lambda e, hd=hd, oc=oc, bk=bk, cur=cur: e.matmul(
                                banks[bk][:, :], lhsT=wbuf[cur][0:64, hd, oc * 128:(oc + 1) * 128], rhs=attnT[:, hd, :],
                                start=(hd == 0), stop=False) for hd in range(16)]
                            fns += [lambda e, c=c, oc=oc, bk=bk, cur=cur: e.matmul(
                                banks[bk][:, :], lhsT=wbuf[cur][:, 16 + c, oc * 128:(oc + 1) * 128], rhs=mixp[:, c, :],
                                start=False, stop=(c == 7)) for c in range(8)]
                            tr.op("tensor", [("attnT", hd, qb) for hd in range(16) for qb in range(4)] +
                                  [("mixp", c) for c in range(8)] + [("wbuf", cur)], [("bank", bk)], fns)
                            tr.op("vector", [("bank", bk), ("hT", dc)], [("hT", dc)],
                                  lambda e, dc=dc, bk=bk: e.tensor_tensor(out=hT[:, dc, :], in0=banks[bk][:, :],
                                                                          in1=hT[:, dc, :], op=OP.add))
                    tr.barrier()
                if DEBUG:
                    tr.dma("sync", "dbg", [("hT", c) for c in range(NCH)], [],
                           lambda e: e.dma_start(out=dbg_d[ps_, 0].rearrange("(c p) t -> p c t", p=128), in_=hT[:]))

                with ExitStack() as p0:
                    qp = sb(p0, "qp", [128, NCH, T], BF16)
                    sq = [sb(p0, f"sqb{i}", [128, T], F32) for i in range(2)]
                    rs = sb(p0, "rsb", [128, T], F32)
                    v12 = sb(p0, "v12", [128, 4, 2, 16], F32)
                    tmp128 = uT[:, 4, 0:512].rearrange("p (h n) -> p h n", h=4)
                    cand = uT[:, 0:2, :].rearrange("p c t -> p (c t)")[:, 0:1024].rearrange("p (h a b) -> p h a b", h=4, a=16)
                    tmp256 = uT[:, 2:4, :].rearrange("p c t -> p (c t)")[:, 0:1024].rearrange("p (h n) -> p h n", h=4)
                    ctop = sb(p0, "ctop", [128, 4, 8, 16], F32)
                    dd = sb(p0, "dd", [128, 32, 16], F32)
                    zz = sb(p0, "zz", [128, 5, 32], F32)

                    rmsnorm("c", hT, "hT", T, 1, xn, "xn", 0, sq, rs)
                    xn_keys = [("xn", c) for c in range(NCH)]
                    nxt = load_w(wbuf, [lambda e, wb: e.dma_start(
                        out=wb[:, 0:NCH, :], in_=wquery_d[:, 0:512].rearrange("(c p) n -> p c n", p=128))])
                    def emit_topk_chains(pairs):
                        chains = []
                        for hl, (tt, h) in enumerate(pairs):
                            ops = []
                            for a in range(2):
                                ops.append(([("s_all", tt, h // 2)], [("v12", hl, a, 0)],
                                            lambda e, a=a, h=h, hl=hl, tt=tt: e.max(out=v12[:, hl, a, 0:8],
                                                                                  in_=s_all[:, tt, h, a, :])))
                                ops.append(([("s_all", tt, h // 2), ("v12", hl, a, 0)], [("tmp128", hl)],
                                            lambda e, a=a, h=h, hl=hl, tt=tt: e.match_replace(
                                                out=tmp128[:, hl, :], in_to_replace=v12[:, hl, a, 0:8],
                                                in_values=s_all[:, tt, h, a, :], imm_value=-1e30)))
                                ops.append(([("tmp128", hl)], [("v12", hl, a, 1)],
                                            lambda e, a=a, hl=hl: e.max(out=v12[:, hl, a, 8:16], in_=tmp128[:, hl, :])))
                            ops.append(([("v12", hl, a, i) for a in range(2) for i in range(2)], [("cand", hl)],
                                        lambda e, hl=hl: e.tensor_tensor(
                                            out=cand[:, hl, :, :],
                                            in0=v12[:, hl, 0, :].unsqueeze(2).to_broadcast([128, 16, 16]),
                                            in1=v12[:, hl, 1, :].unsqueeze(1).to_broadcast([128, 16, 16]), op=OP.add)))
                            ops.append(([("cand", hl)], [("ctop", tt, h, 0)],
                                        lambda e, h=h, hl=hl, tt=tt: e.max(out=ctop[:, tt, h, 0:8],
                                                                           in_=cand[:, hl, :, :].rearrange("p a b -> p (a b)"))))
                            ops.append(([("cand", hl), ("ctop", tt, h, 0)], [("tmp256", hl)],
                                        lambda e, h=h, hl=hl, tt=tt: e.match_replace(
                                            out=tmp256[:, hl, :], in_to_replace=ctop[:, tt, h, 0:8],
                                            in_values=cand[:, hl, :, :].rearrange("p a b -> p (a b)"), imm_value=-1e30)))
                            ops.append(([("tmp256", hl)], [("ctop", tt, h)],
                                        lambda e, h=h, hl=hl, tt=tt: e.max(out=ctop[:, tt, h, 8:16], in_=tmp256[:, hl, :])))
                            chains.append(ops)
                        for i in range(len(chains[0])):
                            for ops in chains:
                                r_, w_, f_ = ops[i]
                                tr.op("vector", r_, w_, f_)

                    for cg in range(4):
                        cur = nxt
                        if cg < 3:
                            nxt = load_w(wbuf, [lambda e, wb, cg=cg: e.dma_start(
                                out=wb[:, 0:NCH, :],
                                in_=wquery_d[:, 512 * (cg + 1):512 * (cg + 2)].rearrange("(c p) n -> p c n", p=128))])
                        for oc in range(4):
                            fc = cg * 4 + oc
                            bk = 1 + fc % 2
                            tr.op("tensor", xn_keys + [("wbuf", cur)], [("bank", bk)],
                                  [lambda e, c=c, oc=oc, bk=bk, cur=cur: e.matmul(
                                      banks[bk][:, :], lhsT=wbuf[cur][:, c, oc * 128:(oc + 1) * 128], rhs=xn[:, c, :],
                                      start=(c == 0), stop=(c == NCH - 1)) for c in range(NCH)])
                            tr.op("scalar", [("bank", bk)], [("qp", fc)], act_copy(qp[:, fc, :], banks[bk][:, :]))
                        b4 = cg
                        for tt in range(4):
                            bk = 3 + tt
                            tr.op("tensor", [("qp", fc) for fc in range(4 * b4, 4 * b4 + 4)] + ["skT"], [("bank", bk)],
                                  [lambda e, j=j, b4=b4, bk=bk, tt=tt: e.matmul(
                                      banks[bk][:, j * 128:(j + 1) * 128],
                                      lhsT=qp[:, 4 * b4 + j, tt * 128:(tt + 1) * 128], rhs=skT[:, j % 2, :],
                                      start=True, stop=True) for j in range(4)])
                            tr.op("scalar", [("bank", bk)], [("s_all", tt, b4)],
                                  lambda e, tt=tt, b4=b4, bk=bk: e.activation(
                                      out=s_all[:, tt, 2 * b4:2 * b4 + 2, :, :],
                                      in_=banks[bk][:, :].rearrange("p (h a n) -> p h a n", h=2, a=2), func=AF.Copy))
                        for tp in range(2):
                            emit_topk_chains([(2 * tp + t2, 2 * cg + hh) for t2 in range(2) for hh in range(2)])

                    ck = [("ctop", tt, h) for tt in range(4) for h in range(8)] + \
                         [("ctop", tt, h, 0) for tt in range(4) for h in range(8)]
                    c3 = ctop[:].rearrange("p t h k -> p (t h) k")
                    cmax = ctop[:, :, :, 0:1].rearrange("p t h o -> p (t h o)")
                    cthr = ctop[:, :, :, 15:16].rearrange("p t h o -> p (t h o)")
                    sall_s1 = s_all[:].rearrange("p t h a n -> p (t h) a n")[:, :, 0, :]
                    all_s = [("s_all", tt, b4) for tt in range(4) for b4 in range(4)]
                    tr.op("vector", ck, ["dd"],
                          lambda e: e.tensor_tensor(out=dd[:], in0=c3,
                                                    in1=ctop[:, :, :, 0:1].rearrange("p t h o -> p (t h) o").to_broadcast([128, 32, 16]),
                                                    op=OP.subtract))
                    tr.op("scalar", ["dd"], ["dd"], lambda e: e.activation(out=dd[:], in_=dd[:], func=AF.Exp))
                    tr.op("vector", ["dd"], [("zz", 0)],
                          lambda e: e.tensor_reduce(out=zz[:, 0, :], in_=dd[:], axis=AX.X, op=OP.add))
                    tr.op("scalar", [("zz", 0)], [("zz", 1)],
                          lambda e: e.activation(out=zz[:, 1, :], in_=zz[:, 0, :], func=AF.Ln))
                    tr.op("vector", [("zz", 1)] + ck, [("zz", 2)],
                          lambda e: e.tensor_tensor(out=zz[:, 2, :], in0=zz[:, 1, :], in1=cmax, op=OP.add))
                    tr.op("vector", ck, [("zz", 3)],
                          lambda e: e.tensor_scalar(out=zz[:, 3, :], in0=cthr, scalar1=-MARGIN, scalar2=None, op0=OP.add))
                    tr.op("vector", [("zz", 3), ("zz", 2)], [("zz", 4)],
                          lambda e: e.tensor_tensor(out=zz[:, 4, :], in0=zz[:, 3, :], in1=zz[:, 2, :], op=OP.subtract))
                    tr.op("scalar", [("zz", 4)], [("tcm", tt) for tt in range(4)],
                          lambda e: e.activation(out=tcm[:].rearrange("p t h -> p (t h)"), in_=zz[:, 4, :], func=AF.Exp))
                    tr.op("vector", [("zz", 3)] + all_s, all_s,
                          lambda e: e.tensor_tensor(
                              out=sall_s1, in0=sall_s1,
                              in1=zz[:, 3, :].unsqueeze(2).to_broadcast([128, 32, 128]), op=OP.subtract))
                    tr.op("vector", [("tcm", tt) for tt in range(4)] + ["ident"],
                          [("dg", tt, h) for tt in range(4) for h in range(8)],
                          lambda e: e.tensor_tensor(
                              out=dg[:].rearrange("p t h n -> p (t h) n"),
                              in0=ident[:].unsqueeze(1).to_broadcast([128, 32, 128]),
                              in1=tcm[:].rearrange("p t h -> p (t h)").unsqueeze(2).to_broadcast([128, 32, 128]), op=OP.mult))
                    tr.barrier()

            with ExitStack() as pp:
                ubuf = [sb(pp, f"ubuf{i}", [128, NCH, 512], BF16) for i in range(2)]
                vbuf = [sb(pp, f"vbuf{i}", [128, 4, D], BF16) for i in range(2)]
                gl = [sb(pp, f"gl{i}", [128, 4, T], BF16) for i in range(2)]
                GT = [sb(pp, f"GT{i}", [128, 4, T], BF16) for i in range(2)]
                NDB = 4
                Dc = [sb(pp, f"Dc{i}", [128, 2, 4, 128], BF16 if i < 3 else F32) for i in range(NDB)]
                Eb = [sb(pp, f"Eb{i}", [128, 2, 512], BF16) for i in range(NDB)]
                Wb = [sb(pp, f"Wb{i}", [128, 8, 512], BF16) for i in range(2)]
                xn_keys = [("xn", c) for c in range(NCH)]
                vbanks = [banks[3][:, :], banks[4][:, :], banks[5][:, :], banks[6][:, :]]
                vbkeys = [("bank", 3), ("bank", 4), ("bank", 5), ("bank", 6)]
                vall = psall[:, 3 * 512:7 * 512].rearrange("p (d t) -> p d t", d=4)

                def ld_u(g):
                    s = g % 2
                    if g == 0:
                        for c4 in range(4):
                            tr.dma("gpsimd", f"uq{c4}", [], [("ubuf", s, c4)], lambda e, c4=c4, s=s: e.dma_start(
                                out=ubuf[s][:, :, c4 * 128:(c4 + 1) * 128],
                                in_=UT_d[:, c4 * 128:(c4 + 1) * 128].rearrange("(c p) n -> p c n", p=128)))
                        return
                    tr.dma("gpsimd", f"u{s}", [], [("ubuf", s, c4) for c4 in range(4)], lambda e, g=g, s=s: e.dma_start(
                        out=ubuf[s][:, :, :], in_=UT_d[:, 512 * g:512 * g + 512].rearrange("(c p) n -> p c n", p=128)))

                def ld_v(g):
                    s = g % 2
                    for hv in range(2):
                        tr.dma("gpsimd", f"v{s}", [], [("vbuf", s, hv)], lambda e, g=g, s=s, hv=hv: e.dma_start(
                            out=vbuf[s][:, 2 * hv:2 * hv + 2, :],
                            in_=V_d[512 * g + 256 * hv:512 * g + 256 * hv + 256, :].rearrange("(c p) n -> p c n", p=128)))

                hbank = [banks[0][:, :], ptb[:].bitcast(F32)]
                hbkey = [("bank", 0), "ptb"]

                def emit_Hc_mm(g, c4):
                    s = g % 2
                    hb, hk = hbank[c4 % 2], hbkey[c4 % 2]
                    tr.op("tensor", xn_keys + [("ubuf", s, c4)], [hk],
                          [lambda e, c=c, c4=c4, hb=hb, s=s: e.matmul(
                              hb, lhsT=ubuf[s][:, c, c4 * 128:(c4 + 1) * 128], rhs=xn[:, c, :],
                              start=(c == 0), stop=(c == NCH - 1)) for c in range(NCH)])

                def emit_Hc_gelu(g, c4):
                    s = g % 2
                    hb, hk = hbank[c4 % 2], hbkey[c4 % 2]
                    tr.op("scalar", [hk], [("gl", s, c4)],
                          lambda e, c4=c4, hb=hb, s=s: e.activation(out=gl[s][:, c4, :], in_=hb, func=AF.Gelu))

                def emit_Hc(g, c4):
                    emit_Hc_mm(g, c4)
                    emit_Hc_gelu(g, c4)

                def emit_gate(g, tt):
                    k = 4 * g + tt
                    wb = Wb[k % 2]
                    wk = ("Wb", k % 2)

                    def parts(hp):
                        di = 4 * k + hp
                        return Dc[di % NDB], Eb[di % NDB], ("Dc", di % NDB), ("Eb", di % NDB), 2 * hp

                    def oplus(eng, hp):
                        dcb, ebb, dk, ek, h0 = parts(hp)
                        tr.op(eng, [("s_all", tt, hp)], [dk],
                              lambda e, dcb=dcb, tt=tt, h0=h0, g=g: e.tensor_tensor(
                                  out=dcb[:],
                                  in0=s_all[:, tt, h0:h0 + 2, 0, 4 * g:4 * g + 4].unsqueeze(3).to_broadcast([128, 2, 4, 128]),
                                  in1=s_all[:, tt, h0:h0 + 2, 1, :].unsqueeze(2).to_broadcast([128, 2, 4, 128]),
                                  op=OP.add))

                    def expo(hp):
                        dcb, ebb, dk, ek, h0 = parts(hp)
                        tr.op("scalar", [dk], [ek],
                              lambda e, dcb=dcb, ebb=ebb: e.activation(
                                  out=ebb[:], in_=dcb[:].rearrange("p h i j -> p h (i j)"), func=AF.Exp))

                    def stt(hp):
                        dcb, ebb, dk, ek, h0 = parts(hp)
                        tr.op("vector", [dk, ek], [(wk, hp)],
                              lambda e, dcb=dcb, ebb=ebb, wb=wb, h0=h0: e.scalar_tensor_tensor(
                                  out=wb[:, h0:h0 + 2, :], in0=dcb[:].rearrange("p h i j -> p h (i j)"),
                                  scalar=0.0, in1=ebb[:], op0=OP.is_ge, op1=OP.mult))

                    oplus("vector", 3)
                    oplus("gpsimd", 0)
                    expo(0)
                    expo(3)
                    stt(0)
                    stt(3)
                    for hp in (1, 2):
                        oplus("gpsimd", hp)
                        dcb, ebb, dk, ek, h0 = parts(hp)
                        tr.op("scalar", [dk], [dk],
                              lambda e, dcb=dcb: e.activation(out=dcb[:].rearrange("p h i j -> p h (i j)"),
                                                              in_=dcb[:].rearrange("p h i j -> p h (i j)"),
                                                              func=AF.Prelu, alpha=1.0e6))
                        tr.op("scalar", [dk], [(wk, hp)],
                              lambda e, dcb=dcb, wb=wb, h0=h0: e.activation(
                                  out=wb[:, h0:h0 + 2, :], in_=dcb[:].rearrange("p h i j -> p h (i j)"), func=AF.Exp))
                    bk = 1 + k % 2
                    tr.op("tensor", [(wk, hp) for hp in range(4)] + [("dg", tt, h) for h in range(8)], [("bank", bk)],
                          [lambda e, il=il, h=h, wb=wb, bk=bk, tt=tt: e.matmul(
                              banks[bk][:, il * 128:(il + 1) * 128], lhsT=wb[:, h, il * 128:(il + 1) * 128],
                              rhs=dg[:, tt, h, :], start=(h == 0), stop=(h == 7)) for il in range(4) for h in range(8)])

                def emit_G(g, tt):
                    k = 4 * g + tt
                    s = g % 2
                    bk = 1 + k % 2
                    tr.op("vector", [("bank", bk)] + [("gl", s, c4) for c4 in range(4)], [("GT", s, tt)],
                          lambda e, bk=bk, s=s, tt=tt: e.tensor_tensor(
                              out=GT[s][:, :, tt * 128:(tt + 1) * 128],
                              in0=banks[bk][:, :].rearrange("p (i q) -> p i q", i=4),
                              in1=gl[s][:, :, tt * 128:(tt + 1) * 128], op=OP.mult))

                def emit_Vmm(g, j):
                    s = g % 2
                    for d4 in range(4):
                        dc = 4 * j + d4
                        vb, vk = vbanks[d4], vbkeys[d4]
                        tr.op("tensor", [("GT", s, tt) for tt in range(4)] + [("vbuf", s, 0), ("vbuf", s, 1)], [vk],
                              [lambda e, c4=c4, dc=dc, vb=vb, s=s: e.matmul(
                                  vb, lhsT=vbuf[s][:, c4, dc * 128:(dc + 1) * 128], rhs=GT[s][:, c4, :],
                                  start=(c4 == 0), stop=(c4 == 3)) for c4 in range(4)])

                def emit_Vflush(g, j):
                    tr.op("vector", vbkeys + [("hT", 4 * j + d4) for d4 in range(4)], [("hT", 4 * j + d4) for d4 in range(4)],
                          lambda e, j=j: e.tensor_tensor(out=hT[:, 4 * j:4 * j + 4, :], in0=vall,
                                                         in1=hT[:, 4 * j:4 * j + 4, :], op=OP.add))

                ld_u(0)
                ld_u(1)
                ld_v(0)
                for c4 in range(4):
                    emit_Hc(0, c4)
                NSLOT = 4 * NEXP_GROUPS
                for k in range(NSLOT + 6):
                    g, tt = k // 4, k % 4
                    hc = None
                    if k < NSLOT:
                        if tt >= 1 and g + 1 < NEXP_GROUPS:
                            hc = (g + 1, tt - 1)
                        if tt == 0 and g >= 1:
                            hc = (g, 3)
                        if hc:
                            emit_Hc_mm(*hc)
                        emit_gate(g, tt)
                        if hc and hc[1] % 2 == 1:
                            emit_Hc_gelu(hc[0], hc[1] - 1)
                            emit_Hc_gelu(hc[0], hc[1])
                        if tt == 3 and g + 2 < NEXP_GROUPS:
                            ld_u(g + 2)
                        if tt == 2 and g >= 1:
                            ld_v(g)
                    if 1 <= k <= NSLOT:
                        emit_G((k - 1) // 4, (k - 1) % 4)
                    if 5 <= k < NSLOT + 5:
                        emit_Vflush((k - 5) // 4, (k - 5) % 4)
                    if 4 <= k < NSLOT + 4:
                        emit_Vmm(g - 1, tt)
                tr.barrier()
            if DEBUG:
                tr.dma("sync", "dbg", [("hT", c) for c in range(NCH)], [],
                       lambda e: e.dma_start(out=dbg_d[ps_, 1].rearrange("(c p) t -> p c t", p=128), in_=hT[:]))

            with ExitStack() as pg:
                wbuf = [sb(pg, f"wbufg{i}", [128, NCH, 512], BF16) for i in range(2)]
                wpj = sb(pg, "wpj", [128, 2, D], BF16)
                pTs = sb(pg, "pTs", [128, 2, T], BF16)
                sq = [sb(pg, f"sqg{i}", [128, T], F32) for i in range(2)]
                rs = sb(pg, "rsg", [128, T], F32)
                sg = [sb(pg, f"sg{i}", [128, T], F32) for i in range(2)]
                oT = sb(pg, "oT", [128, NCH, T], F32)

                tr.dma("gpsimd", "c3", [], ["wpj"],
                       lambda e: e.dma_start(out=wpj[:], in_=wproj_d.rearrange("(c p) n -> p c n", p=128)))
                tr.dma("gpsimd", "c3", [], ["pTs"],
                       lambda e: e.dma_start(out=pTs[:], in_=pT_d[ps_].rearrange("(c p) t -> p c t", p=128)))
                rmsnorm("d", hT, "hT", T, 2, xn, "xn", 0, sq, rs)
                xn_keys = [("xn", c) for c in range(NCH)]
                nxt = load_w(wbuf, [lambda e, wb: e.dma_start(
                    out=wb[:, 0:NCH, :], in_=wgate_d[:, 0:512].rearrange("(c p) n -> p c n", p=128))])
                for cg in range(4):
                    cur = nxt
                    if cg < 3:
                        nxt = load_w(wbuf, [lambda e, wb, cg=cg: e.dma_start(
                            out=wb[:, 0:NCH, :],
                            in_=wgate_d[:, 512 * (cg + 1):512 * (cg + 2)].rearrange("(c p) n -> p c n", p=128))])
                    for oc in range(4):
                        dc = cg * 4 + oc
                        bk = 1 + 2 * (dc % 2)
                        bk2 = bk + 1
                        tr.op("tensor", xn_keys + [("wbuf", cur)], [("bank", bk)],
                              [lambda e, c=c, oc=oc, bk=bk, cur=cur: e.matmul(
                                  banks[bk][:, :], lhsT=wbuf[cur][:, c, oc * 128:(oc + 1) * 128], rhs=xn[:, c, :],
                                  start=(c == 0), stop=(c == NCH - 1)) for c in range(NCH)])
                        tr.op("tensor", ["wpj", "pTs"], [("bank", bk2)],
                              [lambda e, c=c, dc=dc, bk2=bk2: e.matmul(
                                  banks[bk2][:, :], lhsT=wpj[:, c, dc * 128:(dc + 1) * 128], rhs=pTs[:, c, :],
                                  start=(c == 0), stop=(c == 1)) for c in range(2)])
                        sgi = sg[dc % 2]
                        tr.op("scalar", [("bank", bk)], [("sg", dc % 2)],
                              lambda e, sgi=sgi, bk=bk: e.activation(out=sgi[:], in_=banks[bk][:, :], func=AF.Sigmoid))
                        tr.op("vector", [("sg", dc % 2), ("bank", bk2)], [("sg", dc % 2)],
                              lambda e, sgi=sgi, bk2=bk2: e.tensor_tensor(out=sgi[:], in0=banks[bk2][:, :], in1=sgi[:],
                                                                          op=OP.mult))
                        tr.op("vector", [("sg", dc % 2), ("hT", dc)], [("hT", dc)],
                              lambda e, sgi=sgi, dc=dc: e.tensor_tensor(out=hT[:, dc, :], in0=hT[:, dc, :], in1=sgi[:],
                                                                        op=OP.add))
                if DEBUG:
                    tr.dma("sync", "dbg", [("hT", c) for c in range(NCH)], [],
                           lambda e: e.dma_start(out=dbg_d[ps_, 2].rearrange("(c p) t -> p c t", p=128), in_=hT[:]))
                rmsnorm("e", hT, "hT", T, 3, oT, "oT", 0, sq, rs)
                for q4 in range(4):
                    tr.dma("sync", "out", [("oT", c) for c in range(4 * q4, 4 * q4 + 4)], [],
                           lambda e, q4=q4: e.dma_start(
                               out=out_d[ps_, 512 * q4:512 * q4 + 512, :].rearrange("(c p) t -> p c t", p=128),
                               in_=oT[:, 4 * q4:4 * q4 + 4, :]))
                tr.barrier()

        tr.flush()
    return nc


_NC_CACHE = {}


def _consts():
    ident = np.eye(128, dtype=np.float32)
    perm = np.zeros((128, 128), np.float32)
    for p in range(128):
        r = p % 64
        if r < 8:
            perm[p + 8, p] = 1.0
        elif r < 16:
            perm[p - 8, p] = 1.0
        else:
            perm[p, p] = 1.0
    inv_freq = 1.0 / (500000.0 ** (np.arange(0, 16, 2, dtype=np.float32) / np.float32(16)))
    ropec = np.zeros((128, 2), np.float32)
    for p in range(128):
        r = p % 64
        if r < 16:
            ropec[p, 0] = inv_freq[r % 8]
            ropec[p, 1] = -1.0 if r < 8 else 1.0
    q = np.arange(128)[:, None]
    kj = np.arange(256)[None, :]
    rel = q + 128 - kj
    band = (rel >= 0) & (rel < 128)
    maskg = np.where(band, 0.0, -1e30).astype(np.float32)
    maskf = np.where(band & (kj >= 128), 0.0, -1e30).astype(np.float32)
    return ident, perm, ropec, maskg, maskf


def kernel(x, p, positions, g_mix, w_in, sinks, w_pool, pool_scale, w_out, g_ffn,
           w_query, sub_keys, expert_u, expert_v, g_ple, w_ple_gate, w_ple_proj, g_final):
    f = lambda a: np.ascontiguousarray(np.asarray(a), dtype=np.float32)
    x = f(x); p = f(p)
    positions = np.asarray(positions).astype(np.int32)
    w_in = f(w_in)[0]
    ident, perm, ropec, maskg, maskf = _consts()

    wq = w_in[:, 0:1024]
    wk = w_in[:, 1024:1280].reshape(D, 4, 64)
    kdup = np.concatenate([wk, wk], axis=2).reshape(D, 512)
    wu = w_in[:, 1536:2560]
    wqku = np.ascontiguousarray(np.concatenate([wq, kdup, wu], axis=1))
    wv = np.ascontiguousarray(w_in[:, 1280:1536])
    gvec = np.stack([f(g_mix)[0], f(g_ffn)[0], f(g_ple)[0], f(g_final)], axis=0)
    gvec = np.ascontiguousarray(gvec.reshape(4, NCH, 128).transpose(2, 0, 1))
    sinksb = np.ascontiguousarray(np.broadcast_to(f(sinks)[0][None, :], (128, 16)))
    pscale = np.ascontiguousarray(f(pool_scale)[0].reshape(8, 128).T)
    skT = np.ascontiguousarray(f(sub_keys)[0].transpose(0, 2, 1))
    UT = np.ascontiguousarray(f(expert_u)[0].T)
    Vx = f(expert_v)[0]

    shared = dict(maskg=maskg, wqku=wqku, wv=wv, wpool=f(w_pool)[0], pscale=pscale, gvec=gvec, sinksb=sinksb,
                  wout=f(w_out)[0], wquery=f(w_query)[0], skT=skT, UT=UT, Vx=Vx, wgate=f(w_ple_gate)[0],
                  wproj=f(w_ple_proj)[0], ident=ident, perm=perm, ropec=ropec)
    in_maps = []
    for c in range(NCORES):
        b = c // 4
        base = (c % 4) * 1024
        xT = np.zeros((NPASS, D, TH), np.float32)
        pT = np.zeros((NPASS, 256, T), np.float32)
        posb = np.zeros((NPASS, 128, TH), np.int32)
        maskp = np.zeros((NPASS, 128, 256), np.float32)
        icnt = np.zeros((NPASS, 128, 4, 16), np.float32)
        for ps_ in range(NPASS):
            st = base + ps_ * T
            xT[ps_, :, HALO:] = x[b, st:st + T, :].T
            posb[ps_, :, HALO:] = positions[b, st:st + T][None, :]
            if st > 0:
                xT[ps_, :, :HALO] = x[b, st - HALO:st, :].T
                posb[ps_, :, :HALO] = positions[b, st - HALO:st][None, :]
                maskp[ps_] = maskg
            else:
                maskp[ps_] = maskf
            pT[ps_] = p[0, b, st:st + T, :].T
            for gi, w in enumerate((2, 4, 8, 16)):
                tpos = st + np.arange(16)
                icnt[ps_, :, gi, :] = (1.0 / np.minimum(tpos + 1, w).astype(np.float32))[None, :]
        d = dict(shared)
        d.update(xT=xT, pT=pT, posb=posb, maskp=maskp, icnt=icnt)
        in_maps.append(d)

    if "nc" not in _NC_CACHE:
        _NC_CACHE["nc"] = build_program()
    nc = _NC_CACHE["nc"]
    res = run_bass_kernel_spmd(nc, in_maps, core_ids=list(range(NCORES)))
    out = np.zeros((2, 4096, D), np.float32)
    for c in range(NCORES):
        b = c // 4
        base = (c % 4) * 1024
        oT = res.results[c]["outT"]
        for ps_ in range(NPASS):
            st = base + ps_ * T
            out[b, st:st + T, :] = oT[ps_].T
    if DEBUG:
        kernel.dbg = [res.results[c]["dbg"] for c in range(NCORES)]
        kernel.dumps = [{k: v for k, v in res.results[c].items() if k.startswith("dbg_")} for c in range(NCORES)]
    return out
```

```python
import math
import types
from contextlib import ExitStack

import numpy as np
import ml_dtypes

import concourse.bass as bass
import concourse.mybir as mybir
from concourse.bass_utils import run_bass_kernel_spmd

F32 = mybir.dt.float32
BF16 = mybir.dt.bfloat16
I32 = mybir.dt.int32
AF = mybir.ActivationFunctionType
OP = mybir.AluOpType
AX = mybir.AxisListType

D = 2048
NCH = 16
T = 512
HALO = 128
TH = T + HALO
NPASS = 2
NCORES = 8
NEXP_GROUPS = 32
SCALE = 64 ** -0.5
EPS = 1e-6
MARGIN = 2e-6
TWO_PI = 2.0 * math.pi

DEBUG = False


def _freeze(f, depth=0):
    if not isinstance(f, types.FunctionType) or depth > 4:
        return f
    cells = None
    if f.__closure__ is not None:
        cl = []
        for c in f.__closure__:
            try:
                cl.append(types.CellType(_freeze(c.cell_contents, depth + 1)))
            except ValueError:
                cl.append(c)
        cells = tuple(cl)
    dfl = f.__defaults__
    if dfl is not None:
        dfl = tuple(_freeze(d, depth + 1) for d in dfl)
    g = types.FunctionType(f.__code__, f.__globals__, f.__name__, dfl, cells)
    g.__kwdefaults__ = f.__kwdefaults__
    return g


class Tracker:
    ENGS = ["tensor", "vector", "scalar", "gpsimd", "sync"]

    def __init__(self, nc, es):
        self.nc = nc
        self.es = es
        self.prog = {e: [] for e in self.ENGS}
        self.sem = {e: es.enter_context(nc.semaphore("sem_" + e)) for e in self.ENGS}
        self.cnt = {e: 0 for e in self.ENGS}
        self.waited = {e: {} for e in self.ENGS}
        self.last_w = {}
        self.readers = {}
        self.dma_sems = {}
        self.dma_tokens = []

    def _wait(self, eng, tok):
        s, v, owner = tok
        if owner == eng and eng == "tensor":
            return
        if owner.startswith("dma:"):
            v = self.dma_sems[owner[4:]][1]
        w = self.waited[eng]
        if w.get(id(s), 0) >= v:
            return
        w[id(s)] = v
        self.prog[eng].append(lambda e, s=s, v=v: e.wait_ge(s, v))

    def _deps(self, eng, reads, writes):
        for k in reads:
            t = self.last_w.get(k)
            if t is not None:
                self._wait(eng, t)
        for k in writes:
            t = self.last_w.get(k)
            if t is not None:
                self._wait(eng, t)
            for t in self.readers.get(k, ()):
                self._wait(eng, t)

    def _record(self, tok, reads, writes):
        for k in reads:
            self.readers.setdefault(k, []).append(tok)
        for k in writes:
            self.last_w[k] = tok
            self.readers[k] = []

    def op(self, eng, reads, writes, fns):
        if not isinstance(fns, (list, tuple)):
            fns = [fns]
        fns = [_freeze(f) for f in fns]
        self._deps(eng, reads, writes)
        self.cnt[eng] += 1
        v = self.cnt[eng]
        s = self.sem[eng]
        for f in fns[:-1]:
            self.prog[eng].append(lambda e, f=f: f(e))
        last = fns[-1]
        self.prog[eng].append(lambda e, f=last, s=s: f(e).then_inc(s, 1))
        self._record((s, v, eng), reads, writes)

    def dma(self, queue, semname, reads, writes, fn):
        semname = queue + "_" + semname
        if semname not in self.dma_sems:
            self.dma_sems[semname] = [self.es.enter_context(self.nc.semaphore("dsem_" + semname)), 0]
        ent = self.dma_sems[semname]
        fn = _freeze(fn)
        self._deps(queue, reads, writes)
        ent[1] += 16
        s, v = ent[0], ent[1]
        self.prog[queue].append(lambda e, f=fn, s=s: f(e).then_inc(s, 16))
        tok = (s, v, "dma:" + semname)
        self._record(tok, reads, writes)
        self.dma_tokens.append(tok)

    def barrier(self):
        toks = [(self.sem[e], self.cnt[e], e) for e in self.ENGS if self.cnt[e] > 0]
        toks += self.dma_tokens
        self.dma_tokens = []
        for e in self.ENGS:
            for t in toks:
                self._wait(e, t)
        self.last_w = {}
        self.readers = {}
        self.flush()

    def flush(self):
        for eng in self.ENGS:
            prog = self.prog[eng]
            if not prog:
                continue
            self.prog[eng] = []

            def body(e, prog=prog):
                for f in prog:
                    f(e)
            getattr(self.block, eng)(body)

    def emit(self, block):
        tr = self

        @block.tensor
        def _(e):
            for f in tr.prog["tensor"]:
                f(e)

        @block.vector
        def _(e):
            for f in tr.prog["vector"]:
                f(e)

        @block.scalar
        def _(e):
            for f in tr.prog["scalar"]:
                f(e)

        @block.gpsimd
        def _(e):
            for f in tr.prog["gpsimd"]:
                f(e)

        @block.sync
        def _(e):
            for f in tr.prog["sync"]:
                f(e)


def build_program():
    nc = bass.Bass("TRN2", target_bir_lowering=False)

    def din(name, shape, dt=F32):
        return nc.dram_tensor(name, list(shape), dt, kind="ExternalInput").ap()

    xT_d = din("xT", [NPASS, D, TH])
    pT_d = din("pT", [NPASS, 256, T])
    pos_d = din("posb", [NPASS, 128, TH], I32)
    maskp_d = din("maskp", [NPASS, 128, 256])
    maskg_d = din("maskg", [128, 256])
    icnt_d = din("icnt", [NPASS, 128, 4, 16])
    wqku_d = din("wqku", [D, 2560])
    wv_d = din("wv", [D, 256])
    wpool_d = din("wpool", [4, 256, 256])
    pscale_d = din("pscale", [128, 8])
    gvec_d = din("gvec", [128, 4, NCH])
    sinks_d = din("sinksb", [128, 16])
    wout_d = din("wout", [D, D])
    wquery_d = din("wquery", [D, D])
    skT_d = din("skT", [2, 128, 128])
    UT_d = din("UT", [D, 16384])
    V_d = din("Vx", [16384, D])
    wgate_d = din("wgate", [D, D])
    wproj_d = din("wproj", [256, D])
    ident_d = din("ident", [128, 128])
    perm_d = din("perm", [128, 128])
    ropec_d = din("ropec", [128, 2])
    out_d = nc.dram_tensor("outT", [NPASS, D, T], F32, kind="ExternalOutput").ap()
    dbg_d = None
    if DEBUG:
        dbg_d = nc.dram_tensor("dbg", [NPASS, 3, D, T], F32, kind="ExternalOutput").ap()

    with ExitStack() as es:
        tr = Tracker(nc, es)

        uniq = [0]

        def sb(stack, name, shape, dt):
            uniq[0] += 1
            return stack.enter_context(nc.sbuf_tensor(f"sb{uniq[0]}_{name}", list(shape), dt))

        ident = sb(es, "ident", [128, 128], BF16)
        permf = sb(es, "permf", [128, 128], F32)
        onesf = sb(es, "onesf", [128, 128], F32)
        epsT = sb(es, "epsT", [128, 1], F32)
        maskg = sb(es, "maskg", [128, 256], BF16)
        maskp = sb(es, "maskp", [128, 256], BF16)
        gvec = sb(es, "gvec", [128, 4, NCH], F32)
        sinks = sb(es, "sinks", [128, 16], F32)
        pscale = sb(es, "pscale", [128, 8], F32)
        ropec = sb(es, "ropec", [128, 2], F32)
        skT = sb(es, "skT", [128, 2, 128], BF16)
        hT = sb(es, "hT", [128, NCH, T], F32)
        xn = sb(es, "xn", [128, NCH, T], BF16)
        s_all = sb(es, "s_all", [128, 4, 8, 2, 128], F32)
        tcm = sb(es, "tcm", [128, 4, 8], F32)
        dg = sb(es, "dg", [128, 4, 8, 128], BF16)

        psall = es.enter_context(nc.psum_tensor("psall", [128, 7 * 512], F32))
        banks = [psall[:, i * 512:(i + 1) * 512] for i in range(7)]
        ptb = es.enter_context(nc.psum_tensor("ptb", [128, 1024], BF16))

        block = es.enter_context(nc.Block())
        tr.block = block

        dumps = {}

        def dump(name, ap, shape, keys, ps_):
            if not DEBUG or ps_ != 0:
                return
            dd_ = nc.dram_tensor("dbg_" + name, list(shape), F32, kind="ExternalOutput").ap()
            dumps[name] = dd_
            tr.dma("gpsimd", "dbgd", keys, [], lambda e: e.dma_start(out=dd_, in_=ap))

        def act_copy(out, in_):
            return lambda e: e.activation(out=out, in_=in_, func=AF.Copy)

        tr.dma("gpsimd", "c", [], ["ident"], lambda e: e.dma_start(out=ident[:], in_=ident_d[:, :]))
        tr.dma("sync", "c", [], ["permf"], lambda e: e.dma_start(out=permf[:], in_=perm_d[:, :]))
        tr.dma("gpsimd", "c", [], ["maskg"], lambda e: e.dma_start(out=maskg[:], in_=maskg_d[:, :]))
        tr.dma("sync", "c", [], ["gvec"], lambda e: e.dma_start(out=gvec[:], in_=gvec_d[:, :, :]))
        tr.dma("sync", "c", [], ["sinks"], lambda e: e.dma_start(out=sinks[:], in_=sinks_d[:, :]))
        tr.dma("sync", "c", [], ["pscale"], lambda e: e.dma_start(out=pscale[:], in_=pscale_d[:, :]))
        tr.dma("sync", "c", [], ["ropec"], lambda e: e.dma_start(out=ropec[:], in_=ropec_d[:, :]))
        tr.dma("gpsimd", "c", [], ["skT"],
               lambda e: e.dma_start(out=skT[:], in_=skT_d.rearrange("a k n -> k a n")))
        tr.op("vector", [], ["onesf"], lambda e: e.memset(onesf[:], 1.0))
        tr.op("vector", [], ["epsT"], lambda e: e.memset(epsT[:], EPS))

        def rmsnorm(stack_tag, src, srckey, n, gi, dst, dstkey, bank_a, sq, rs, col0=0):
            pa = banks[bank_a]
            for c in range(NCH):
                sqc = sq[c % 2]
                tr.op("scalar", [(srckey, c)], [("sq", id(sqc))],
                      lambda e, c=c, sqc=sqc: e.activation(out=sqc[:, 0:n], in_=src[:, c, 0:n], func=AF.Square))
                tr.op("tensor", [("sq", id(sqc)), "onesf"], [("bank", bank_a)],
                      lambda e, c=c, sqc=sqc: e.matmul(pa[:, 0:n], lhsT=onesf[:], rhs=sqc[:, 0:n],
                                                       start=(c == 0), stop=(c == NCH - 1)))
            tr.op("scalar", [("bank", bank_a), "epsT"], [("rs", id(rs))],
                  lambda e: e.activation(out=rs[:, 0:n], in_=pa[:, 0:n], func=AF.Sqrt, bias=epsT[:], scale=1.0 / D))
            tr.op("vector", [("rs", id(rs))], [("rs", id(rs))],
                  lambda e: e.reciprocal(out=rs[:, 0:n], in_=rs[:, 0:n]))
            for c in range(NCH):
                tr.op("vector", [(srckey, c), ("rs", id(rs)), "gvec"], [(dstkey, c)],
                      lambda e, c=c: e.scalar_tensor_tensor(out=dst[:, c, col0:col0 + n], in0=src[:, c, 0:n],
                                                            scalar=gvec[:, gi, c:c + 1], in1=rs[:, 0:n],
                                                            op0=OP.mult, op1=OP.mult))

        wslot_ctr = [0]

        def load_w(wbuf, fn_dma_list):
            s = wslot_ctr[0] % len(wbuf)
            wslot_ctr[0] += 1
            for fn in fn_dma_list:
                tr.dma("gpsimd", f"w{s}", [], [("wbuf", s)], lambda e, fn=fn, s=s: fn(e, wbuf[s]))
            return s

        for ps_ in range(NPASS):
            with ExitStack() as p1:
                wbuf = [sb(p1, f"wbuf{i}", [128, NCH, 512], BF16) for i in range(3)]
                qT = sb(p1, "qT", [128, 8, T], BF16)
                kT = sb(p1, "kT", [128, 4, TH], BF16)
                Vsb = sb(p1, "Vsb", [128, 5, 256], BF16)
                uT = sb(p1, "uT", [128, 8, TH], F32)
                icnt = sb(p1, "icnt", [128, 4, 16], F32)
                wpool = sb(p1, "wpool", [128, 4, 2, 256], BF16)

                tr.dma("gpsimd", "c2", [], ["maskp"], lambda e: e.dma_start(out=maskp[:], in_=maskp_d[ps_, :, :]))
                tr.dma("sync", "c2", [], ["icnt"], lambda e: e.dma_start(out=icnt[:], in_=icnt_d[ps_, :, :, :]))
                tr.dma("gpsimd", "c2", [], ["wpool"],
                       lambda e: e.dma_start(out=wpool[:], in_=wpool_d.rearrange("g (cc p) n -> p g cc n", p=128)))
                for q4 in range(4):
                    tr.dma("sync", "x", [], [("hT", c) for c in range(4 * q4, 4 * q4 + 4)],
                           lambda e, q4=q4: e.dma_start(
                               out=hT[:, 4 * q4:4 * q4 + 4, :],
                               in_=xT_d[ps_, 512 * q4:512 * q4 + 512, HALO:TH].rearrange("(c p) t -> p c t", p=128)))

                with ExitStack() as a1:
                    xh = xn[:].rearrange("p c t -> p (c t)").bitcast(F32)[:, 0:NCH * HALO].rearrange(
                        "p (c t) -> p c t", c=NCH)
                    hn = s_all[:].rearrange("p a b c d -> p (a b c d)").bitcast(BF16)[:, 0:NCH * TH].rearrange(
                        "p (c t) -> p c t", c=NCH)
                    sq = [sb(a1, f"sq{i}", [128, T], F32) for i in range(2)]
                    rs = sb(a1, "rs", [128, T], F32)
                    rs2 = sb(a1, "rs2", [128, HALO], F32)
                    qraw = [sb(a1, f"qraw{i}", [128, TH], F32) for i in range(2)]
                    rt1 = [sb(a1, f"rt1_{i}", [128, TH], F32) for i in range(2)]
                    rt2 = [sb(a1, f"rt2_{i}", [128, TH], F32) for i in range(2)]
                    Ct = sb(a1, "Ct", [128, TH], F32)
                    St = sb(a1, "St", [128, TH], F32)
                    posi = qraw[1][:].bitcast(I32)
                    K_POSI = [("qraw", 1, 0), ("qraw", 1, 1)]
                    ang, ra, rb, ki = rt1[0], rt1[1], rt2[0], posi
                    K_ANG, K_RA = ("rt1", 0), ("rt1", 1)
                    K_RB = [("rt2", 0, 0), ("rt2", 0, 1)]

                    tr.dma("sync", "x", [], [("xh", c) for c in range(NCH)],
                           lambda e: e.dma_start(out=xh[:, :, :], in_=xT_d[ps_, :, 0:HALO].rearrange("(c p) t -> p c t", p=128)))
                    tr.dma("sync", "c2", [], K_POSI, lambda e: e.dma_start(out=posi, in_=pos_d[ps_, :, :]))

                    tr.op("vector", K_POSI, [K_ANG], lambda e: e.tensor_copy(out=ang[:], in_=posi))
                    tr.op("vector", [K_ANG, "ropec"], [K_ANG],
                          lambda e: e.tensor_scalar(out=ang[:], in0=ang[:], scalar1=ropec[:, 0:1], scalar2=None, op0=OP.mult))

                    def sin_of(shift, dst, sgn):
                        tr.op("vector", [K_ANG], [K_RA],
                              lambda e: e.tensor_scalar(out=ra[:], in0=ang[:], scalar1=1.0 / TWO_PI,
                                                        scalar2=shift / TWO_PI, op0=OP.mult, op1=OP.add))
                        tr.op("vector", [K_RA], K_POSI, lambda e: e.tensor_copy(out=ki, in_=ra[:]))
                        tr.op("vector", K_POSI, [K_RA], lambda e: e.tensor_copy(out=ra[:], in_=ki))
                        tr.op("vector", [K_RA], [K_RA],
                              lambda e: e.tensor_scalar(out=ra[:], in0=ra[:], scalar1=-TWO_PI, scalar2=shift,
                                                        op0=OP.mult, op1=OP.add))
                        tr.op("vector", [K_RA, K_ANG], K_RB,
                              lambda e: e.tensor_tensor(out=rb[:], in0=ra[:], in1=ang[:], op=OP.add))
                        tr.op("vector", K_RB, [K_RA],
                              lambda e: e.tensor_scalar(out=ra[:], in0=rb[:], scalar1=math.pi, scalar2=-TWO_PI,
                                                        op0=OP.is_gt, op1=OP.mult))
                        tr.op("vector", [K_RA] + K_RB, K_RB,
                              lambda e: e.tensor_tensor(out=rb[:], in0=rb[:], in1=ra[:], op=OP.add))
                        tr.op("vector", K_RB, [K_RA],
                              lambda e: e.tensor_scalar(out=ra[:], in0=rb[:], scalar1=-math.pi, scalar2=TWO_PI,
                                                        op0=OP.is_lt, op1=OP.mult))
                        tr.op("vector", [K_RA] + K_RB, K_RB,
                              lambda e: e.tensor_tensor(out=rb[:], in0=rb[:], in1=ra[:], op=OP.add))
                        tr.op("vector", K_RB, K_RB,
                              lambda e: e.tensor_scalar(out=rb[:], in0=rb[:], scalar1=3.1415925, scalar2=-3.1415925,
                                                        op0=OP.min, op1=OP.max))
                        tr.op("scalar", K_RB, [dst[1]], lambda e: e.activation(out=dst[0][:], in_=rb[:], func=AF.Sin))
                        if sgn:
                            tr.op("vector", [dst[1], "ropec"], [dst[1]],
                                  lambda e: e.tensor_scalar(out=dst[0][:], in0=dst[0][:], scalar1=ropec[:, 1:2],
                                                            scalar2=None, op0=OP.mult))

                    sin_of(0.0, (St, "St"), True)
                    sin_of(math.pi / 2, (Ct, "Ct"), False)

                    rmsnorm("a", hT, "hT", T, 0, hn, "hn_o", 0, sq, rs, col0=HALO)
                    rmsnorm("b", xh, "xh", HALO, 0, hn, "hn_h", 1, sq, rs2, col0=0)
                    hn_keys = [("hn_o", c) for c in range(NCH)] + [("hn_h", c) for c in range(NCH)]

                    sv = load_w(wbuf, [lambda e, wb: e.dma_start(
                        out=wb[:, 0:NCH, 0:256], in_=wv_d.rearrange("(c p) n -> p c n", p=128))])
                    nxt = load_w(wbuf, [lambda e, wb: e.dma_start(
                        out=wb[:, 0:NCH, :], in_=wqku_d[:, 0:512].rearrange("(c p) n -> p c n", p=128))])
                    for tt in range(5):
                        bk = 2 + tt % 2
                        tr.op("tensor", hn_keys + [("wbuf", sv)], [("bank", bk)],
                              [lambda e, c=c, tt=tt, bk=bk: e.matmul(banks[bk][:, 0:256],
                                                                      lhsT=hn[:, c, tt * 128:(tt + 1) * 128],
                                                                      rhs=wbuf[sv][:, c, 0:256],
                                                                      start=(c == 0), stop=(c == NCH - 1))
                               for c in range(NCH)])
                        tr.op("scalar", [("bank", bk)], [("Vsb", tt)],
                              act_copy(Vsb[:, tt, :], banks[bk][:, 0:256]))

                    ev = 0
                    for cg in range(5):
                        cur = nxt
                        if cg < 4:
                            nxt = load_w(wbuf, [lambda e, wb, cg=cg: e.dma_start(
                                out=wb[:, 0:NCH, :],
                                in_=wqku_d[:, 512 * (cg + 1):512 * (cg + 2)].rearrange("(c p) n -> p c n", p=128))])
                        for oc in range(4):
                            kind = "q" if cg < 2 else ("k" if cg == 2 else "u")
                            ba, bb = (2, 3) if ev % 2 == 0 else (4, 5)
                            ev += 1
                            tr.op("tensor", hn_keys + [("wbuf", cur)], [("bank", ba)],
                                  [lambda e, c=c, oc=oc, ba=ba, cur=cur: e.matmul(
                                      banks[ba][:, :], lhsT=wbuf[cur][:, c, oc * 128:(oc + 1) * 128],
                                      rhs=hn[:, c, HALO:TH], start=(c == 0), stop=(c == NCH - 1))
                                   for c in range(NCH)])
                            if kind != "q":
                                tr.op("tensor", hn_keys + [("wbuf", cur)], [("bank", bb)],
                                      [lambda e, c=c, oc=oc, bb=bb, cur=cur: e.matmul(
                                          banks[bb][:, 0:HALO], lhsT=wbuf[cur][:, c, oc * 128:(oc + 1) * 128],
                                          rhs=hn[:, c, 0:HALO], start=(c == 0), stop=(c == NCH - 1))
                                       for c in range(NCH)])
                            if kind == "u":
                                ch = (cg - 3) * 4 + oc
                                tr.op("scalar", [("bank", ba)], [("uT", ch, 1)],
                                      act_copy(uT[:, ch, HALO:TH], banks[ba][:, :]))
                                tr.op("scalar", [("bank", bb)], [("uT", ch, 0)],
                                      act_copy(uT[:, ch, 0:HALO], banks[bb][:, 0:HALO]))
                                continue
                            qi = ev % 2
                            qr = qraw[qi]
                            lo = HALO if kind == "q" else 0
                            tr.op("scalar", [("bank", ba)], [("qraw", qi, 1)], act_copy(qr[:, HALO:TH], banks[ba][:, :]))
                            if kind == "k":
                                tr.op("scalar", [("bank", bb)], [("qraw", qi, 0)],
                                      act_copy(qr[:, 0:HALO], banks[bb][:, 0:HALO]))
                            tr.op("tensor", [("qraw", qi, 1), "permf"], [("bank", ba)],
                                  lambda e, qr=qr, ba=ba: e.matmul(banks[ba][:, :], lhsT=permf[:], rhs=qr[:, HALO:TH],
                                                                   start=True, stop=True))
                            if kind == "k":
                                tr.op("tensor", [("qraw", qi, 0), "permf"], [("bank", bb)],
                                      lambda e, qr=qr, bb=bb: e.matmul(banks[bb][:, 0:HALO], lhsT=permf[:],
                                                                       rhs=qr[:, 0:HALO], start=True, stop=True))
                            t1 = rt1[qi]
                            t2 = rt2[qi]
                            tr.op("gpsimd", [("qraw", qi, 1), ("qraw", qi, 0), "Ct"], [("rt1", qi)],
                                  lambda e, qr=qr, t1=t1, lo=lo: e.tensor_tensor(out=t1[:, lo:TH], in0=qr[:, lo:TH],
                                                                                in1=Ct[:, lo:TH], op=OP.mult))
                            tr.op("vector", [("bank", ba), "St"], [("rt2", qi, 1)],
                                  lambda e, t2=t2, ba=ba: e.tensor_tensor(out=t2[:, HALO:TH], in0=banks[ba][:, :],
                                                                          in1=St[:, HALO:TH], op=OP.mult))
                            if kind == "k":
                                tr.op("vector", [("bank", bb), "St"], [("rt2", qi, 0)],
                                      lambda e, t2=t2, bb=bb: e.tensor_tensor(out=t2[:, 0:HALO], in0=banks[bb][:, 0:HALO],
                                                                              in1=St[:, 0:HALO], op=OP.mult))
                            if kind == "q":
                                ch = cg * 4 + oc
                                tr.op("gpsimd", [("rt1", qi), ("rt2", qi, 1)], [("qT", ch)],
                                      lambda e, t1=t1, t2=t2, ch=ch: e.tensor_tensor(out=qT[:, ch, :], in0=t1[:, HALO:TH],
                                                                                    in1=t2[:, HALO:TH], op=OP.add))
                            else:
                                tr.op("gpsimd", [("rt1", qi), ("rt2", qi, 1), ("rt2", qi, 0)], [("kT", oc)],
                                      lambda e, t1=t1, t2=t2, oc=oc: e.tensor_tensor(out=kT[:, oc, :], in0=t1[:, :],
                                                                                    in1=t2[:, :], op=OP.add))
                    dump("hn", hn, [128, NCH, TH], hn_keys, ps_)
                    dump("qT", qT[:], [128, 8, T], [("qT", c) for c in range(8)], ps_)
                    dump("kT", kT[:], [128, 4, TH], [("kT", c) for c in range(4)], ps_)
                    dump("Vsb", Vsb[:], [128, 5, 256], [("Vsb", c) for c in range(5)], ps_)
                    dump("uT", uT[:], [128, 8, TH], [("uT", c, i) for c in range(8) for i in range(2)], ps_)
                    dump("Ct", Ct[:], [128, TH], ["Ct"], ps_)
                    dump("St", St[:], [128, TH], ["St"], ps_)
                    tr.barrier()

                with ExitStack() as a2:
                    Pf = [sb(a2, f"Pf{i}", [128, 4, 256], F32) for i in range(2)]
                    Pn = [sb(a2, f"Pn{i}", [128, 4, 256], BF16) for i in range(2)]
                    PTs = [sb(a2, f"PTs{i}", [128, 1024], BF16) for i in range(2)]
                    sm = [sb(a2, f"sm{i}", [128, 8, 4], F32) for i in range(2)]
                    nsinks = sb(a2, "nsinks", [128, 16], F32)
                    attnT = s_all[:].rearrange("p a b c d -> p (a b c d)").bitcast(BF16)[:, 0:8 * T].rearrange(
                        "p (h t) -> p h t", h=8)
                    pooled = xn[:, 0:8, :]
                    mixp = xn[:, 8:16, :]
                    pa_ = sb(a2, "pa_", [128, TH], F32)
                    pb_ = sb(a2, "pb_", [128, TH], F32)

                    def attn_front(it):
                        qb, g = it // 4, it % 4
                        msk, mkey = (maskp, "maskp") if qb == 0 else (maskg, "maskg")
                        pi = it % 2
                        b0, b1 = (0, 1) if pi == 0 else (2, 3)
                        smt = sm[pi]
                        fns = []
                        for hh in range(4):
                            hd = 4 * g + hh
                            bp = (hd % 2) * 64
                            bk = b0 if hh < 2 else b1
                            co = (hh % 2) * 256
                            fns.append(lambda e, hd=hd, bp=bp, bk=bk, co=co, qb=qb, g=g: e.matmul(
                                banks[bk][:, co:co + 256], lhsT=qT[bp:bp + 64, hd // 2, qb * 128:(qb + 1) * 128],
                                rhs=kT[bp:bp + 64, g, qb * 128:qb * 128 + 256], start=True, stop=False))
                            fns.append(lambda e, bk=bk, co=co, msk=msk: e.matmul(
                                banks[bk][:, co:co + 256], lhsT=ident[:], rhs=msk[:], start=False, stop=True))
                        tr.op("tensor", [("qT", c) for c in (2 * g, 2 * g + 1)] + [("kT", g), "ident", mkey],
                              [("bank", b0), ("bank", b1)], fns)
                        for half, bk in ((0, b0), (1, b1)):
                            tr.op("vector", [("bank", bk)], [("sm", pi, "mx", half)],
                                  lambda e, bk=bk, half=half, smt=smt: e.tensor_reduce(
                                      out=smt[:, 0, 2 * half:2 * half + 2],
                                      in_=banks[bk][:, :].rearrange("p (h k) -> p h k", h=2), axis=AX.X, op=OP.max))
                        tr.op("vector", [("sm", pi, "mx", 0), ("sm", pi, "mx", 1), "sinks"], [("sm", pi, "m2")],
                              lambda e, smt=smt, g=g: e.scalar_tensor_tensor(
                                  out=smt[:, 2, :], in0=smt[:, 0, :], scalar=-SCALE, in1=nsinks[:, 4 * g:4 * g + 4],
                                  op0=OP.mult, op1=OP.min))
                        tr.op("vector", [("sm", pi, "m2"), "sinks"], [("sm", pi, "m")],
                              lambda e, smt=smt, g=g: e.tensor_tensor(out=smt[:, 3, :], in0=smt[:, 2, :],
                                                                      in1=sinks[:, 4 * g:4 * g + 4], op=OP.add))
                        pf = Pf[pi]
                        fns = []
                        for hh in range(4):
                            bk = b0 if hh < 2 else b1
                            co = (hh % 2) * 256
                            fns.append(lambda e, hh=hh, bk=bk, co=co, pf=pf, smt=smt: e.activation(
                                out=pf[:, hh, :], in_=banks[bk][:, co:co + 256], func=AF.Exp,
                                bias=smt[:, 2, hh:hh + 1], scale=SCALE, accum_out=smt[:, 4, hh:hh + 1]))
                        fns.append(lambda e, smt=smt: e.activation(out=smt[:, 5, :], in_=smt[:, 3, :], func=AF.Exp))
                        tr.op("scalar", [("bank", b0), ("bank", b1), ("sm", pi, "m"), ("sm", pi, "m2")],
                              [("Pf", pi), ("sm", pi, "z")], fns)

                    def attn_back(it):
                        qb, g = it // 4, it % 4
                        pi = it % 2
                        bo = 4 + pi
                        smt = sm[pi]
                        pf = Pf[pi]
                        pn = Pn[pi]
                        tr.op("vector", [("sm", pi, "z")], [("sm", pi, "zz")],
                              lambda e, smt=smt: e.tensor_tensor(out=smt[:, 6, :], in0=smt[:, 4, :], in1=smt[:, 5, :],
                                                                 op=OP.add))
                        tr.op("vector", [("sm", pi, "zz")], [("sm", pi, "rz")],
                              lambda e, smt=smt: e.reciprocal(out=smt[:, 7, :], in_=smt[:, 6, :]))
                        tr.op("vector", [("Pf", pi), ("sm", pi, "rz")], [("Pn", pi)],
                              lambda e, smt=smt, pf=pf, pn=pn: e.tensor_tensor(
                                  out=pn[:], in0=pf[:], in1=smt[:, 7, :].unsqueeze(2).to_broadcast([128, 4, 256]),
                                  op=OP.mult))
                        fns = []
                        for kb in range(2):
                            for hh in range(4):
                                o = (kb * 4 + hh) * 128
                                fns.append(lambda e, kb=kb, hh=hh, o=o, pn=pn: e.transpose(
                                    out=ptb[:, o:o + 128], in_=pn[:, hh, kb * 128:(kb + 1) * 128], identity=ident[:]))
                        tr.op("tensor", [("Pn", pi), "ident"], ["ptb"], fns)
                        pts = PTs[pi]
                        tr.op("scalar", ["ptb"], [("PTs", pi)], act_copy(pts[:], ptb[:]))
                        tr.op("tensor", [("PTs", pi), ("Vsb", qb), ("Vsb", qb + 1)], [("bank", bo)],
                              [lambda e, kb=kb, par=par, pts=pts, bo=bo, qb=qb, g=g: e.matmul(
                                  banks[bo][par * 64:(par + 1) * 64, 0:256], lhsT=Vsb[:, qb + kb, g * 64:(g + 1) * 64],
                                  rhs=pts[:, kb * 512:(kb + 1) * 512].rearrange("p (a b q) -> p a b q", a=2, b=2)[:, :, par, :],
                                  start=(kb == 0), stop=(kb == 1))
                               for par in range(2) for kb in range(2)])
                        tr.op("vector", [("bank", bo)], [("attnT", 2 * g + a, qb) for a in range(2)],
                              lambda e, bo=bo, qb=qb, g=g: e.tensor_copy(
                                  out=attnT[:, 2 * g:2 * g + 2, qb * 128:(qb + 1) * 128],
                                  in_=banks[bo][:, 0:256].rearrange("p (a q) -> p a q", a=2)))

                    tr.op("vector", ["sinks"], ["nsinks"],
                          lambda e: e.tensor_scalar(out=nsinks[:], in0=sinks[:], scalar1=-1.0, scalar2=None, op0=OP.mult))
                    attn_front(0)
                    for it in range(16):
                        if it + 1 < 16:
                            attn_front(it + 1)
                        attn_back(it)

                    for ch in range(8):
                        gi = ch // 2
                        w = 2 ** (gi + 1)
                        src = None
                        bufs = [pa_, pb_]
                        names = ["pa_", "pb_"]
                        cur_ap, cur_key = None, None
                        sft = 1
                        lvl = 0
                        while sft < w:
                            dst = bufs[lvl % 2]
                            dk = names[lvl % 2]
                            lo_ = HALO - 17 + 2 * sft
                            if lvl == 0:
                                tr.op("gpsimd", [("uT", ch, 0), ("uT", ch, 1)], [dk],
                                      lambda e, dst=dst, sft=sft, ch=ch, lo_=lo_: e.tensor_tensor(
                                          out=dst[:, lo_:TH], in0=uT[:, ch, lo_:TH], in1=uT[:, ch, lo_ - sft:TH - sft], op=OP.add))
                            else:
                                srcb = bufs[(lvl - 1) % 2]
                                sk_ = names[(lvl - 1) % 2]
                                tr.op("gpsimd", [sk_], [dk],
                                      lambda e, dst=dst, srcb=srcb, sft=sft, lo_=lo_: e.tensor_tensor(
                                          out=dst[:, lo_:TH], in0=srcb[:, lo_:TH], in1=srcb[:, lo_ - sft:TH - sft], op=OP.add))
                            cur_ap, cur_key = dst, dk
                            sft *= 2
                            lvl += 1
                        tr.op("gpsimd", [cur_key, "icnt"], [cur_key],
                              lambda e, cur_ap=cur_ap, gi=gi: e.tensor_tensor(
                                  out=cur_ap[:, HALO:HALO + 16], in0=cur_ap[:, HALO:HALO + 16], in1=icnt[:, gi, :], op=OP.mult))
                        tr.op("gpsimd", [cur_key], [cur_key],
                              lambda e, cur_ap=cur_ap, w=w: e.tensor_scalar(
                                  out=cur_ap[:, HALO + 16:TH], in0=cur_ap[:, HALO + 16:TH], scalar1=1.0 / w, scalar2=None,
                                  op0=OP.mult))
                        tr.op("gpsimd", [cur_key, ("uT", ch, 1)], [("pooled", ch)],
                              lambda e, cur_ap=cur_ap, ch=ch: e.tensor_tensor(
                                  out=pooled[:, ch, :], in0=cur_ap[:, HALO:TH], in1=uT[:, ch, HALO:TH], op=OP.subtract))
                    for ch in range(8):
                        gi, ec = ch // 2, ch % 2
                        bk = ch % 2
                        tr.op("tensor", [("pooled", 2 * gi), ("pooled", 2 * gi + 1), "wpool"], [("bank", bk)],
                              [lambda e, cc=cc, gi=gi, ec=ec, bk=bk: e.matmul(
                                  banks[bk][:, :], lhsT=wpool[:, gi, cc, ec * 128:(ec + 1) * 128],
                                  rhs=pooled[:, 2 * gi + cc, :], start=(cc == 0), stop=(cc == 1)) for cc in range(2)])
                        tr.op("scalar", [("bank", bk), "pscale"], [("mixp", ch)],
                              lambda e, ch=ch, bk=bk: e.activation(out=mixp[:, ch, :], in_=banks[bk][:, :], func=AF.Copy,
                                                                   scale=pscale[:, ch:ch + 1]))

                    dump("attnT", attnT, [128, 8, T], [("attnT", pr, qb) for pr in range(8) for qb in range(4)], ps_)
                    dump("pooled", pooled, [128, 8, T], [("pooled", c) for c in range(8)], ps_)
                    dump("mixp", mixp, [128, 8, T], [("mixp", c) for c in range(8)], ps_)

                    def ld_wout(cg):
                        return load_w(wbuf, [lambda e, wb, cg=cg: e.dma_start(
                            out=wb[:, 0:NCH, :],
                            in_=wout_d[:, 512 * cg:512 * cg + 512].rearrange("(c p) n -> p c n", p=128))])

                    nxt = ld_wout(0)
                    for cg in range(4):
                        cur = nxt
                        if cg < 3:
                            nxt = ld_wout(cg + 1)
                        for oc in range(4):
                            dc = cg * 4 + oc
                            bk = 2 + dc % 2
                            fns = [lambda e, pr=pr, oc=oc, bk=bk, cur=cur: e.matmul(
                                banks[bk][:, :], lhsT=wbuf[cur][:, pr, oc * 128:(oc + 1) * 128], rhs=attnT[:, pr, :],
                                start=(pr == 0), stop=False) for pr in range(8)]
                            fns += [lambda e, c=c, oc=oc, bk=bk, cur=cur: e.matmul(
                                banks[bk][:, :], lhsT=wbuf[cur][:, 8 + c, oc * 128:(oc + 1) * 128], rhs=mixp[:, c, :],
                                start=False, stop=(c == 7)) for c in range(8)]
                            tr.op("tensor", [("attnT", pr, qb) for pr in range(8) for qb in range(4)] +
                                  [("mixp", c) for c in range(8)] + [("wbuf", cur)], [("bank", bk)], fns)
                            tr.op("vector", [("bank", bk), ("hT", dc)], [("hT", dc)],
                                  lambda e, dc=dc, bk=bk: e.tensor_tensor(out=hT[:, dc, :], in0=banks[bk][:, :],
                                                                          in1=hT[:, dc, :], op=OP.add))
                    tr.barrier()
                if DEBUG:
                    tr.dma("sync", "dbg", [("hT", c) for c in range(NCH)], [],
                           lambda e: e.dma_start(out=dbg_d[ps_, 0].rearrange("(c p) t -> p c t", p=128), in_=hT[:]))

                with ExitStack() as p0:
                    qp = sb(p0, "qp", [128, NCH, T], BF16)
                    sq = [sb(p0, f"sqb{i}", [128, T], F32) for i in range(2)]
                    rs = sb(p0, "rsb", [128, T], F32)
                    v12 = sb(p0, "v12", [128, 4, 2, 16], F32)
                    tmp128 = uT[:, 4, 0:512].rearrange("p (h n) -> p h n", h=4)
                    cand = uT[:, 0:2, :].rearrange("p c t -> p (c t)")[:, 0:1024].rearrange("p (h a b) -> p h a b", h=4, a=16)
                    tmp256 = uT[:, 2:4, :].rearrange("p c t -> p (c t)")[:, 0:1024].rearrange("p (h n) -> p h n", h=4)
                    ctop = sb(p0, "ctop", [128, 4, 8, 16], F32)
                    dd = sb(p0, "dd", [128, 8, 16], F32)
                    zz = sb(p0, "zz", [128, 5, 8], F32)

                    rmsnorm("c", hT, "hT", T, 1, xn, "xn", 0, sq, rs)
                    xn_keys = [("xn", c) for c in range(NCH)]
                    nxt = load_w(wbuf, [lambda e, wb: e.dma_start(
                        out=wb[:, 0:NCH, :], in_=wquery_d[:, 0:512].rearrange("(c p) n -> p c n", p=128))])
                    def emit_topk_chains(pairs):
                        chains = []
                        for hl, (tt, h) in enumerate(pairs):
                            ops = []
                            for a in range(2):
                                ops.append(([("s_all", tt, h // 2)], [("v12", hl, a, 0)],
                                            lambda e, a=a, h=h, hl=hl, tt=tt: e.max(out=v12[:, hl, a, 0:8],
                                                                                  in_=s_all[:, tt, h, a, :])))
                                ops.append(([("s_all", tt, h // 2), ("v12", hl, a, 0)], [("tmp128", hl)],
                                            lambda e, a=a, h=h, hl=hl, tt=tt: e.match_replace(
                                                out=tmp128[:, hl, :], in_to_replace=v12[:, hl, a, 0:8],
                                                in_values=s_all[:, tt, h, a, :], imm_value=-1e30)))
                                ops.append(([("tmp128", hl)], [("v12", hl, a, 1)],
                                            lambda e, a=a, hl=hl: e.max(out=v12[:, hl, a, 8:16], in_=tmp128[:, hl, :])))
                            ops.append(([("v12", hl, a, i) for a in range(2) for i in range(2)], [("cand", hl)],
                                        lambda e, hl=hl: e.tensor_tensor(
                                            out=cand[:, hl, :, :],
                                            in0=v12[:, hl, 0, :].unsqueeze(2).to_broadcast([128, 16, 16]),
                                            in1=v12[:, hl, 1, :].unsqueeze(1).to_broadcast([128, 16, 16]), op=OP.add)))
                            ops.append(([("cand", hl)], [("ctop", tt, h, 0)],
                                        lambda e, h=h, hl=hl, tt=tt: e.max(out=ctop[:, tt, h, 0:8],
                                                                           in_=cand[:, hl, :, :].rearrange("p a b -> p (a b)"))))
                            ops.append(([("cand", hl), ("ctop", tt, h, 0)], [("tmp256", hl)],
                                        lambda e, h=h, hl=hl, tt=tt: e.match_replace(
                                            out=tmp256[:, hl, :], in_to_replace=ctop[:, tt, h, 0:8],
                                            in_values=cand[:, hl, :, :].rearrange("p a b -> p (a b)"), imm_value=-1e30)))
                            ops.append(([("tmp256", hl)], [("ctop", tt, h)],
                                        lambda e, h=h, hl=hl, tt=tt: e.max(out=ctop[:, tt, h, 8:16], in_=tmp256[:, hl, :])))
                            chains.append(ops)
                        for i in range(len(chains[0])):
                            for ops in chains:
                                r_, w_, f_ = ops[i]
                                tr.op("vector", r_, w_, f_)

                    for cg in range(4):
                        cur = nxt
                        if cg < 3:
                            nxt = load_w(wbuf, [lambda e, wb, cg=cg: e.dma_start(
                                out=wb[:, 0:NCH, :],
                                in_=wquery_d[:, 512 * (cg + 1):512 * (cg + 2)].rearrange("(c p) n -> p c n", p=128))])
                        for oc in range(4):
                            fc = cg * 4 + oc
                            bk = 1 + fc % 2
                            tr.op("tensor", xn_keys + [("wbuf", cur)], [("bank", bk)],
                                  [lambda e, c=c, oc=oc, bk=bk, cur=cur: e.matmul(
                                      banks[bk][:, :], lhsT=wbuf[cur][:, c, oc * 128:(oc + 1) * 128], rhs=xn[:, c, :],
                                      start=(c == 0), stop=(c == NCH - 1)) for c in range(NCH)])
                            tr.op("scalar", [("bank", bk)], [("qp", fc)], act_copy(qp[:, fc, :], banks[bk][:, :]))
                        b4 = cg
                        for tt in range(4):
                            bk = 3 + tt
                            tr.op("tensor", [("qp", fc) for fc in range(4 * b4, 4 * b4 + 4)] + ["skT"], [("bank", bk)],
                                  [lambda e, j=j, b4=b4, bk=bk, tt=tt: e.matmul(
                                      banks[bk][:, j * 128:(j + 1) * 128],
                                      lhsT=qp[:, 4 * b4 + j, tt * 128:(tt + 1) * 128], rhs=skT[:, j % 2, :],
                                      start=True, stop=True) for j in range(4)])
                            tr.op("scalar", [("bank", bk)], [("s_all", tt, b4)],
                                  lambda e, tt=tt, b4=b4, bk=bk: e.activation(
                                      out=s_all[:, tt, 2 * b4:2 * b4 + 2, :, :],
                                      in_=banks[bk][:, :].rearrange("p (h a n) -> p h a n", h=2, a=2), func=AF.Copy))
                        for tp in range(2):
                            emit_topk_chains([(2 * tp + t2, 2 * cg + hh) for t2 in range(2) for hh in range(2)])

                    ctop_all = ctop
                    for tt in range(4):
                        ctop = ctop_all[:, tt, :, :]
                        ck = [("ctop", tt, h) for h in range(8)] + [("ctop", tt, h, 0) for h in range(8)]
                        tr.op("vector", ck, ["dd"],
                              lambda e: e.tensor_tensor(out=dd[:], in0=ctop[:],
                                                        in1=ctop[:, :, 0:1].to_broadcast([128, 8, 16]), op=OP.subtract))
                        tr.op("scalar", ["dd"], ["dd"], lambda e: e.activation(out=dd[:], in_=dd[:], func=AF.Exp))
                        tr.op("vector", ["dd"], [("zz", 0)],
                              lambda e: e.tensor_reduce(out=zz[:, 0, :], in_=dd[:], axis=AX.X, op=OP.add))
                        tr.op("scalar", [("zz", 0)], [("zz", 1)],
                              lambda e: e.activation(out=zz[:, 1, :], in_=zz[:, 0, :], func=AF.Ln))
                        tr.op("vector", [("zz", 1)] + ck, [("zz", 2)],
                              lambda e: e.tensor_tensor(out=zz[:, 2, :], in0=zz[:, 1, :],
                                                        in1=ctop[:, :, 0:1].rearrange("p h o -> p (h o)"), op=OP.add))
                        tr.op("vector", ck, [("zz", 3)],
                              lambda e: e.tensor_scalar(out=zz[:, 3, :], in0=ctop[:, :, 15:16].rearrange("p h o -> p (h o)"),
                                                        scalar1=-MARGIN, scalar2=None, op0=OP.add))
                        tr.op("vector", [("zz", 3), ("zz", 2)], [("zz", 4)],
                              lambda e: e.tensor_tensor(out=zz[:, 4, :], in0=zz[:, 3, :], in1=zz[:, 2, :], op=OP.subtract))
                        tr.op("scalar", [("zz", 4)], [("tcm", tt)],
                              lambda e, tt=tt: e.activation(out=tcm[:, tt, :], in_=zz[:, 4, :], func=AF.Exp))
                        tr.op("vector", [("zz", 3)] + [("s_all", tt, b4) for b4 in range(4)],
                              [("s_all", tt, b4) for b4 in range(4)],
                              lambda e, tt=tt: e.tensor_tensor(
                                  out=s_all[:, tt, :, 0, :], in0=s_all[:, tt, :, 0, :],
                                  in1=zz[:, 3, :].unsqueeze(2).to_broadcast([128, 8, 128]), op=OP.subtract))
                        tr.op("vector", [("tcm", tt), "ident"], [("dg", tt, h) for h in range(8)],
                              lambda e, tt=tt: e.tensor_tensor(
                                  out=dg[:, tt, :, :], in0=ident[:].unsqueeze(1).to_broadcast([128, 8, 128]),
                                  in1=tcm[:, tt, :].unsqueeze(2).to_broadcast([128, 8, 128]), op=OP.mult))
                    tr.barrier()

            with ExitStack() as pp:
                ubuf = [sb(pp, f"ubuf{i}", [128, NCH, 512], BF16) for i in range(2)]
                vbuf = [sb(pp, f"vbuf{i}", [128, 4, D], BF16) for i in range(2)]
                gl = [sb(pp, f"gl{i}", [128, 4, T], BF16) for i in range(2)]
                GT = [sb(pp, f"GT{i}", [128, 4, T], BF16) for i in range(2)]
                NDB = 4
                Dc = [sb(pp, f"Dc{i}", [128, 2, 4, 128], BF16 if i < 3 else F32) for i in range(NDB)]
                Eb = [sb(pp, f"Eb{i}", [128, 2, 512], BF16) for i in range(NDB)]
                Wb = [sb(pp, f"Wb{i}", [128, 8, 512], BF16) for i in range(2)]
                xn_keys = [("xn", c) for c in range(NCH)]
                vbanks = [banks[3][:, :], banks[4][:, :], banks[5][:, :], banks[6][:, :]]
                vbkeys = [("bank", 3), ("bank", 4), ("bank", 5), ("bank", 6)]
                vall = psall[:, 3 * 512:7 * 512].rearrange("p (d t) -> p d t", d=4)

                def ld_u(g):
                    s = g % 2
                    if g == 0:
                        for c4 in range(4):
                            tr.dma("gpsimd", f"uq{c4}", [], [("ubuf", s, c4)], lambda e, c4=c4, s=s: e.dma_start(
                                out=ubuf[s][:, :, c4 * 128:(c4 + 1) * 128],
                                in_=UT_d[:, c4 * 128:(c4 + 1) * 128].rearrange("(c p) n -> p c n", p=128)))
                        return
                    tr.dma("gpsimd", f"u{s}", [], [("ubuf", s, c4) for c4 in range(4)], lambda e, g=g, s=s: e.dma_start(
                        out=ubuf[s][:, :, :], in_=UT_d[:, 512 * g:512 * g + 512].rearrange("(c p) n -> p c n", p=128)))

                def ld_v(g):
                    s = g % 2
                    for hv in range(2):
                        tr.dma("gpsimd", f"v{s}", [], [("vbuf", s, hv)], lambda e, g=g, s=s, hv=hv: e.dma_start(
                            out=vbuf[s][:, 2 * hv:2 * hv + 2, :],
                            in_=V_d[512 * g + 256 * hv:512 * g + 256 * hv + 256, :].rearrange("(c p) n -> p c n", p=128)))

                hbank = [banks[0][:, :], ptb[:].bitcast(F32)]
                hbkey = [("bank", 0), "ptb"]

                def emit_Hc_mm(g, c4):
                    s = g % 2
                    hb, hk = hbank[c4 % 2], hbkey[c4 % 2]
                    tr.op("tensor", xn_keys + [("ubuf", s, c4)], [hk],
                          [lambda e, c=c, c4=c4, hb=hb, s=s: e.matmul(
                              hb, lhsT=ubuf[s][:, c, c4 * 128:(c4 + 1) * 128], rhs=xn[:, c, :],
                              start=(c == 0), stop=(c == NCH - 1)) for c in range(NCH)])

                def emit_Hc_gelu(g, c4):
                    s = g % 2
                    hb, hk = hbank[c4 % 2], hbkey[c4 % 2]
                    tr.op("scalar", [hk], [("gl", s, c4)],
                          lambda e, c4=c4, hb=hb, s=s: e.activation(out=gl[s][:, c4, :], in_=hb, func=AF.Gelu))

                def emit_Hc(g, c4):
                    emit_Hc_mm(g, c4)
                    emit_Hc_gelu(g, c4)

                def emit_gate(g, tt):
                    k = 4 * g + tt
                    wb = Wb[k % 2]
                    wk = ("Wb", k % 2)

                    def parts(hp):
                        di = 4 * k + hp
                        return Dc[di % NDB], Eb[di % NDB], ("Dc", di % NDB), ("Eb", di % NDB), 2 * hp

                    def oplus(eng, hp):
                        dcb, ebb, dk, ek, h0 = parts(hp)
                        tr.op(eng, [("s_all", tt, hp)], [dk],
                              lambda e, dcb=dcb, tt=tt, h0=h0, g=g: e.tensor_tensor(
                                  out=dcb[:],
                                  in0=s_all[:, tt, h0:h0 + 2, 0, 4 * g:4 * g + 4].unsqueeze(3).to_broadcast([128, 2, 4, 128]),
                                  in1=s_all[:, tt, h0:h0 + 2, 1, :].unsqueeze(2).to_broadcast([128, 2, 4, 128]),
                                  op=OP.add))

                    def expo(hp):
                        dcb, ebb, dk, ek, h0 = parts(hp)
                        tr.op("scalar", [dk], [ek],
                              lambda e, dcb=dcb, ebb=ebb: e.activation(
                                  out=ebb[:], in_=dcb[:].rearrange("p h i j -> p h (i j)"), func=AF.Exp))

                    def stt(hp):
                        dcb, ebb, dk, ek, h0 = parts(hp)
                        tr.op("vector", [dk, ek], [(wk, hp)],
                              lambda e, dcb=dcb, ebb=ebb, wb=wb, h0=h0: e.scalar_tensor_tensor(
                                  out=wb[:, h0:h0 + 2, :], in0=dcb[:].rearrange("p h i j -> p h (i j)"),
                                  scalar=0.0, in1=ebb[:], op0=OP.is_ge, op1=OP.mult))

                    oplus("vector", 3)
                    oplus("gpsimd", 0)
                    expo(0)
                    expo(3)
                    stt(0)
                    stt(3)
                    for hp in (1, 2):
                        oplus("gpsimd", hp)
                        dcb, ebb, dk, ek, h0 = parts(hp)
                        tr.op("scalar", [dk], [dk],
                              lambda e, dcb=dcb: e.activation(out=dcb[:].rearrange("p h i j -> p h (i j)"),
                                                              in_=dcb[:].rearrange("p h i j -> p h (i j)"),
                                                              func=AF.Prelu, alpha=1.0e6))
                        tr.op("scalar", [dk], [(wk, hp)],
                              lambda e, dcb=dcb, wb=wb, h0=h0: e.activation(
                                  out=wb[:, h0:h0 + 2, :], in_=dcb[:].rearrange("p h i j -> p h (i j)"), func=AF.Exp))
                    bk = 1 + k % 2
                    tr.op("tensor", [(wk, hp) for hp in range(4)] + [("dg", tt, h) for h in range(8)], [("bank", bk)],
                          [lambda e, il=il, h=h, wb=wb, bk=bk, tt=tt: e.matmul(
                              banks[bk][:, il * 128:(il + 1) * 128], lhsT=wb[:, h, il * 128:(il + 1) * 128],
                              rhs=dg[:, tt, h, :], start=(h == 0), stop=(h == 7)) for il in range(4) for h in range(8)])

                def emit_G(g, tt):
                    k = 4 * g + tt
                    s = g % 2
                    bk = 1 + k % 2
                    tr.op("vector", [("bank", bk)] + [("gl", s, c4) for c4 in range(4)], [("GT", s, tt)],
                          lambda e, bk=bk, s=s, tt=tt: e.tensor_tensor(
                              out=GT[s][:, :, tt * 128:(tt + 1) * 128],
                              in0=banks[bk][:, :].rearrange("p (i q) -> p i q", i=4),
                              in1=gl[s][:, :, tt * 128:(tt + 1) * 128], op=OP.mult))

                def emit_Vmm(g, j):
                    s = g % 2
                    for d4 in range(4):
                        dc = 4 * j + d4
                        vb, vk = vbanks[d4], vbkeys[d4]
                        tr.op("tensor", [("GT", s, tt) for tt in range(4)] + [("vbuf", s, 0), ("vbuf", s, 1)], [vk],
                              [lambda e, c4=c4, dc=dc, vb=vb, s=s: e.matmul(
                                  vb, lhsT=vbuf[s][:, c4, dc * 128:(dc + 1) * 128], rhs=GT[s][:, c4, :],
                                  start=(c4 == 0), stop=(c4 == 3)) for c4 in range(4)])

                def emit_Vflush(g, j):
                    tr.op("vector", vbkeys + [("hT", 4 * j + d4) for d4 in range(4)], [("hT", 4 * j + d4) for d4 in range(4)],
                          lambda e, j=j: e.tensor_tensor(out=hT[:, 4 * j:4 * j + 4, :], in0=vall,
                                                         in1=hT[:, 4 * j:4 * j + 4, :], op=OP.add))

                ld_u(0)
                ld_u(1)
                ld_v(0)
                for c4 in range(4):
                    emit_Hc(0, c4)
                NSLOT = 4 * NEXP_GROUPS
                for k in range(NSLOT + 6):
                    g, tt = k // 4, k % 4
                    hc = None
                    if k < NSLOT:
                        if tt >= 1 and g + 1 < NEXP_GROUPS:
                            hc = (g + 1, tt - 1)
                        if tt == 0 and g >= 1:
                            hc = (g, 3)
                        if hc:
                            emit_Hc_mm(*hc)
                        emit_gate(g, tt)
                        if hc and hc[1] % 2 == 1:
                            emit_Hc_gelu(hc[0], hc[1] - 1)
                            emit_Hc_gelu(hc[0], hc[1])
                        if tt == 3 and g + 2 < NEXP_GROUPS:
                            ld_u(g + 2)
                        if tt == 2 and g >= 1:
                            ld_v(g)
                    if 1 <= k <= NSLOT:
                        emit_G((k - 1) // 4, (k - 1) % 4)
                    if 5 <= k < NSLOT + 5:
                        emit_Vflush((k - 5) // 4, (k - 5) % 4)
                    if 4 <= k < NSLOT + 4:
                        emit_Vmm(g - 1, tt)
                tr.barrier()
            if DEBUG:
                tr.dma("sync", "dbg", [("hT", c) for c in range(NCH)], [],
                       lambda e: e.dma_start(out=dbg_d[ps_, 1].rearrange("(c p) t -> p c t", p=128), in_=hT[:]))

            with ExitStack() as pg:
                wbuf = [sb(pg, f"wbufg{i}", [128, NCH, 512], BF16) for i in range(2)]
                wpj = sb(pg, "wpj", [128, 2, D], BF16)
                pTs = sb(pg, "pTs", [128, 2, T], BF16)
                sq = [sb(pg, f"sqg{i}", [128, T], F32) for i in range(2)]
                rs = sb(pg, "rsg", [128, T], F32)
                sg = [sb(pg, f"sg{i}", [128, T], F32) for i in range(2)]
                oT = sb(pg, "oT", [128, NCH, T], F32)

                tr.dma("gpsimd", "c3", [], ["wpj"],
                       lambda e: e.dma_start(out=wpj[:], in_=wproj_d.rearrange("(c p) n -> p c n", p=128)))
                tr.dma("gpsimd", "c3", [], ["pTs"],
                       lambda e: e.dma_start(out=pTs[:], in_=pT_d[ps_].rearrange("(c p) t -> p c t", p=128)))
                rmsnorm("d", hT, "hT", T, 2, xn, "xn", 0, sq, rs)
                xn_keys = [("xn", c) for c in range(NCH)]
                nxt = load_w(wbuf, [lambda e, wb: e.dma_start(
                    out=wb[:, 0:NCH, :], in_=wgate_d[:, 0:512].rearrange("(c p) n -> p c n", p=128))])
                for cg in range(4):
                    cur = nxt
                    if cg < 3:
                        nxt = load_w(wbuf, [lambda e, wb, cg=cg: e.dma_start(
                            out=wb[:, 0:NCH, :],
                            in_=wgate_d[:, 512 * (cg + 1):512 * (cg + 2)].rearrange("(c p) n -> p c n", p=128))])
                    for oc in range(4):
                        dc = cg * 4 + oc
                        bk = 1 + 2 * (dc % 2)
                        bk2 = bk + 1
                        tr.op("tensor", xn_keys + [("wbuf", cur)], [("bank", bk)],
                              [lambda e, c=c, oc=oc, bk=bk, cur=cur: e.matmul(
                                  banks[bk][:, :], lhsT=wbuf[cur][:, c, oc * 128:(oc + 1) * 128], rhs=xn[:, c, :],
                                  start=(c == 0), stop=(c == NCH - 1)) for c in range(NCH)])
                        tr.op("tensor", ["wpj", "pTs"], [("bank", bk2)],
                              [lambda e, c=c, dc=dc, bk2=bk2: e.matmul(
                                  banks[bk2][:, :], lhsT=wpj[:, c, dc * 128:(dc + 1) * 128], rhs=pTs[:, c, :],
                                  start=(c == 0), stop=(c == 1)) for c in range(2)])
                        sgi = sg[dc % 2]
                        tr.op("scalar", [("bank", bk)], [("sg", dc % 2)],
                              lambda e, sgi=sgi, bk=bk: e.activation(out=sgi[:], in_=banks[bk][:, :], func=AF.Sigmoid))
                        tr.op("vector", [("sg", dc % 2), ("bank", bk2)], [("sg", dc % 2)],
                              lambda e, sgi=sgi, bk2=bk2: e.tensor_tensor(out=sgi[:], in0=banks[bk2][:, :], in1=sgi[:],
                                                                          op=OP.mult))
                        tr.op("vector", [("sg", dc % 2), ("hT", dc)], [("hT", dc)],
                              lambda e, sgi=sgi, dc=dc: e.tensor_tensor(out=hT[:, dc, :], in0=hT[:, dc, :], in1=sgi[:],
                                                                        op=OP.add))
                if DEBUG:
                    tr.dma("sync", "dbg", [("hT", c) for c in range(NCH)], [],
                           lambda e: e.dma_start(out=dbg_d[ps_, 2].rearrange("(c p) t -> p c t", p=128), in_=hT[:]))
                rmsnorm("e", hT, "hT", T, 3, oT, "oT", 0, sq, rs)
                for q4 in range(4):
                    tr.dma("sync", "out", [("oT", c) for c in range(4 * q4, 4 * q4 + 4)], [],
                           lambda e, q4=q4: e.dma_start(
                               out=out_d[ps_, 512 * q4:512 * q4 + 512, :].rearrange("(c p) t -> p c t", p=128),
                               in_=oT[:, 4 * q4:4 * q4 + 4, :]))
                tr.barrier()

        tr.flush()
    return nc


_NC_CACHE = {}


def _consts():
    ident = np.eye(128, dtype=np.float32)
    perm = np.zeros((128, 128), np.float32)
    for p in range(128):
        r = p % 64
        if r < 8:
            perm[p + 8, p] = 1.0
        elif r < 16:
            perm[p - 8, p] = 1.0
        else:
            perm[p, p] = 1.0
    inv_freq = 1.0 / (500000.0 ** (np.arange(0, 16, 2, dtype=np.float32) / np.float32(16)))
    ropec = np.zeros((128, 2), np.float32)
    for p in range(128):
        r = p % 64
        if r < 16:
            ropec[p, 0] = inv_freq[r % 8]
            ropec[p, 1] = -1.0 if r < 8 else 1.0
    q = np.arange(128)[:, None]
    kj = np.arange(256)[None, :]
    rel = q + 128 - kj
    band = (rel >= 0) & (rel < 128)
    maskg = np.where(band, 0.0, -1e30).astype(np.float32)
    maskf = np.where(band & (kj >= 128), 0.0, -1e30).astype(np.float32)
    return ident, perm, ropec, maskg, maskf


def kernel(x, p, positions, g_mix, w_in, sinks, w_pool, pool_scale, w_out, g_ffn,
           w_query, sub_keys, expert_u, expert_v, g_ple, w_ple_gate, w_ple_proj, g_final):
    f = lambda a: np.ascontiguousarray(np.asarray(a), dtype=np.float32)
    x = f(x); p = f(p)
    positions = np.asarray(positions).astype(np.int32)
    w_in = f(w_in)[0]
    ident, perm, ropec, maskg, maskf = _consts()

    wq = w_in[:, 0:1024]
    wk = w_in[:, 1024:1280].reshape(D, 4, 64)
    kdup = np.concatenate([wk, wk], axis=2).reshape(D, 512)
    wu = w_in[:, 1536:2560]
    wqku = np.ascontiguousarray(np.concatenate([wq, kdup, wu], axis=1))
    wv = np.ascontiguousarray(w_in[:, 1280:1536])
    gvec = np.stack([f(g_mix)[0], f(g_ffn)[0], f(g_ple)[0], f(g_final)], axis=0)
    gvec = np.ascontiguousarray(gvec.reshape(4, NCH, 128).transpose(2, 0, 1))
    sinksb = np.ascontiguousarray(np.broadcast_to(f(sinks)[0][None, :], (128, 16)))
    pscale = np.ascontiguousarray(f(pool_scale)[0].reshape(8, 128).T)
    skT = np.ascontiguousarray(f(sub_keys)[0].transpose(0, 2, 1))
    UT = np.ascontiguousarray(f(expert_u)[0].T)
    Vx = f(expert_v)[0]

    shared = dict(maskg=maskg, wqku=wqku, wv=wv, wpool=f(w_pool)[0], pscale=pscale, gvec=gvec, sinksb=sinksb,
                  wout=f(w_out)[0], wquery=f(w_query)[0], skT=skT, UT=UT, Vx=Vx, wgate=f(w_ple_gate)[0],
                  wproj=f(w_ple_proj)[0], ident=ident, perm=perm, ropec=ropec)
    in_maps = []
    for c in range(NCORES):
        b = c // 4
        base = (c % 4) * 1024
        xT = np.zeros((NPASS, D, TH), np.float32)
        pT = np.zeros((NPASS, 256, T), np.float32)
        posb = np.zeros((NPASS, 128, TH), np.int32)
        maskp = np.zeros((NPASS, 128, 256), np.float32)
        icnt = np.zeros((NPASS, 128, 4, 16), np.float32)
        for ps_ in range(NPASS):
            st = base + ps_ * T
            xT[ps_, :, HALO:] = x[b, st:st + T, :].T
            posb[ps_, :, HALO:] = positions[b, st:st + T][None, :]
            if st > 0:
                xT[ps_, :, :HALO] = x[b, st - HALO:st, :].T
                posb[ps_, :, :HALO] = positions[b, st - HALO:st][None, :]
                maskp[ps_] = maskg
            else:
                maskp[ps_] = maskf
            pT[ps_] = p[0, b, st:st + T, :].T
            for gi, w in enumerate((2, 4, 8, 16)):
                tpos = st + np.arange(16)
                icnt[ps_, :, gi, :] = (1.0 / np.minimum(tpos + 1, w).astype(np.float32))[None, :]
        d = dict(shared)
        d.update(xT=xT, pT=pT, posb=posb, maskp=maskp, icnt=icnt)
        in_maps.append(d)

    if "nc" not in _NC_CACHE:
        _NC_CACHE["nc"] = build_program()
    nc = _NC_CACHE["nc"]
    res = run_bass_kernel_spmd(nc, in_maps, core_ids=list(range(NCORES)))
    out = np.zeros((2, 4096, D), np.float32)
    for c in range(NCORES):
        b = c // 4
        base = (c % 4) * 1024
        oT = res.results[c]["outT"]
        for ps_ in range(NPASS):
            st = base + ps_ * T
            out[b, st:st + T, :] = oT[ps_].T
    if DEBUG:
        kernel.dbg = [res.results[c]["dbg"] for c in range(NCORES)]
        kernel.dumps = [{k: v for k, v in res.results[c].items() if k.startswith("dbg_")} for c in range(NCORES)]
    return out
```

```python
import math
import types
from contextlib import ExitStack

import numpy as np
import ml_dtypes

import concourse.bass as bass
import concourse.mybir as mybir
from concourse.bass_utils import run_bass_kernel_spmd

F32 = mybir.dt.float32
BF16 = mybir.dt.bfloat16
I32 = mybir.dt.int32
AF = mybir.ActivationFunctionType
OP = mybir.AluOpType
AX = mybir.AxisListType

D = 2048
NCH = 16
T = 512
HALO = 128
TH = T + HALO
NPASS = 2
NCORES = 8
NEXP_GROUPS = 32
SCALE = 64 ** -0.5
EPS = 1e-6
MARGIN = 2e-6
TWO_PI = 2.0 * math.pi

DEBUG = False


def _freeze(f, depth=0):
    if not isinstance(f, types.FunctionType) or depth > 4:
        return f
    cells = None
    if f.__closure__ is not None:
        cl = []
        for c in f.__closure__:
            try:
                cl.append(types.CellType(_freeze(c.cell_contents, depth + 1)))
            except ValueError:
                cl.append(c)
        cells = tuple(cl)
    dfl = f.__defaults__
    if dfl is not None:
        dfl = tuple(_freeze(d, depth + 1) for d in dfl)
    g = types.FunctionType(f.__code__, f.__globals__, f.__name__, dfl, cells)
    g.__kwdefaults__ = f.__kwdefaults__
    return g


class Tracker:
    ENGS = ["tensor", "vector", "scalar", "gpsimd", "sync"]

    def __init__(self, nc, es):
        self.nc = nc
        self.es = es
        self.prog = {e: [] for e in self.ENGS}
        self.sem = {e: es.enter_context(nc.semaphore("sem_" + e)) for e in self.ENGS}
        self.cnt = {e: 0 for e in self.ENGS}
        self.waited = {e: {} for e in self.ENGS}
        self.last_w = {}
        self.readers = {}
        self.dma_sems = {}
        self.dma_tokens = []

    def _wait(self, eng, tok):
        s, v, owner = tok
        if owner == eng and eng == "tensor":
            return
        if owner.startswith("dma:"):
            v = self.dma_sems[owner[4:]][1]
        w = self.waited[eng]
        if w.get(id(s), 0) >= v:
            return
        w[id(s)] = v
        self.prog[eng].append(lambda e, s=s, v=v: e.wait_ge(s, v))

    def _deps(self, eng, reads, writes):
        for k in reads:
            t = self.last_w.get(k)
            if t is not None:
                self._wait(eng, t)
        for k in writes:
            t = self.last_w.get(k)
            if t is not None:
                self._wait(eng, t)
            for t in self.readers.get(k, ()):
                self._wait(eng, t)

    def _record(self, tok, reads, writes):
        for k in reads:
            self.readers.setdefault(k, []).append(tok)
        for k in writes:
            self.last_w[k] = tok
            self.readers[k] = []

    def op(self, eng, reads, writes, fns):
        if not isinstance(fns, (list, tuple)):
            fns = [fns]
        fns = [_freeze(f) for f in fns]
        self._deps(eng, reads, writes)
        self.cnt[eng] += 1
        v = self.cnt[eng]
        s = self.sem[eng]
        for f in fns[:-1]:
            self.prog[eng].append(lambda e, f=f: f(e))
        last = fns[-1]
        self.prog[eng].append(lambda e, f=last, s=s: f(e).then_inc(s, 1))
        self._record((s, v, eng), reads, writes)

    def dma(self, queue, semname, reads, writes, fn):
        semname = queue + "_" + semname
        if semname not in self.dma_sems:
            self.dma_sems[semname] = [self.es.enter_context(self.nc.semaphore("dsem_" + semname)), 0]
        ent = self.dma_sems[semname]
        fn = _freeze(fn)
        self._deps(queue, reads, writes)
        ent[1] += 16
        s, v = ent[0], ent[1]
        self.prog[queue].append(lambda e, f=fn, s=s: f(e).then_inc(s, 16))
        tok = (s, v, "dma:" + semname)
        self._record(tok, reads, writes)
        self.dma_tokens.append(tok)

    def barrier(self):
        toks = [(self.sem[e], self.cnt[e], e) for e in self.ENGS if self.cnt[e] > 0]
        toks += self.dma_tokens
        self.dma_tokens = []
        for e in self.ENGS:
            for t in toks:
                self._wait(e, t)
        self.last_w = {}
        self.readers = {}
        self.flush()

    def flush(self):
        for eng in self.ENGS:
            prog = self.prog[eng]
            if not prog:
                continue
            self.prog[eng] = []

            def body(e, prog=prog):
                for f in prog:
                    f(e)
            getattr(self.block, eng)(body)

    def emit(self, block):
        tr = self

        @block.tensor
        def _(e):
            for f in tr.prog["tensor"]:
                f(e)

        @block.vector
        def _(e):
            for f in tr.prog["vector"]:
                f(e)

        @block.scalar
        def _(e):
            for f in tr.prog["scalar"]:
                f(e)

        @block.gpsimd
        def _(e):
            for f in tr.prog["gpsimd"]:
                f(e)

        @block.sync
        def _(e):
            for f in tr.prog["sync"]:
                f(e)


def build_program():
    nc = bass.Bass("TRN2", target_bir_lowering=False)

    def din(name, shape, dt=F32):
        return nc.dram_tensor(name, list(shape), dt, kind="ExternalInput").ap()

    xT_d = din("xT", [NPASS, D, TH])
    pT_d = din("pT", [NPASS, 256, T])
    pos_d = din("posb", [NPASS, 128, TH], I32)
    maskp_d = din("maskp", [NPASS, 128, 256])
    maskg_d = din("maskg", [128, 256])
    icnt_d = din("icnt", [NPASS, 128, 4, 16])
    wqku_d = din("wqku", [D, 2560])
    wv_d = din("wv", [D, 256])
    wpool_d = din("wpool", [4, 256, 256])
    pscale_d = din("pscale", [128, 8])
    gvec_d = din("gvec", [128, 4, NCH])
    sinks_d = din("sinksb", [128, 16])
    wout_d = din("wout", [D, D])
    wquery_d = din("wquery", [D, D])
    skT_d = din("skT", [2, 128, 128])
    UT_d = din("UT", [D, 16384])
    V_d = din("Vx", [16384, D])
    wgate_d = din("wgate", [D, D])
    wproj_d = din("wproj", [256, D])
    ident_d = din("ident", [128, 128])
    perm_d = din("perm", [128, 128])
    ropec_d = din("ropec", [128, 2])
    out_d = nc.dram_tensor("outT", [NPASS, D, T], F32, kind="ExternalOutput").ap()
    dbg_d = None
    if DEBUG:
        dbg_d = nc.dram_tensor("dbg", [NPASS, 3, D, T], F32, kind="ExternalOutput").ap()

    with ExitStack() as es:
        tr = Tracker(nc, es)

        uniq = [0]

        def sb(stack, name, shape, dt):
            uniq[0] += 1
            return stack.enter_context(nc.sbuf_tensor(f"sb{uniq[0]}_{name}", list(shape), dt))

        ident = sb(es, "ident", [128, 128], BF16)
        permf = sb(es, "permf", [128, 128], F32)
        onesf = sb(es, "onesf", [128, 128], F32)
        epsT = sb(es, "epsT", [128, 1], F32)
        maskg = sb(es, "maskg", [128, 256], BF16)
        maskp = sb(es, "maskp", [128, 256], BF16)
        gvec = sb(es, "gvec", [128, 4, NCH], F32)
        sinks = sb(es, "sinks", [128, 16], F32)
        pscale = sb(es, "pscale", [128, 8], F32)
        ropec = sb(es, "ropec", [128, 2], F32)
        skT = sb(es, "skT", [128, 2, 128], BF16)
        hT = sb(es, "hT", [128, NCH, T], F32)
        xn = sb(es, "xn", [128, NCH, T], BF16)
        s_all = sb(es, "s_all", [128, 4, 8, 2, 128], F32)
        tcm = sb(es, "tcm", [128, 4, 8], F32)
        dg = sb(es, "dg", [128, 4, 8, 128], BF16)

        psall = es.enter_context(nc.psum_tensor("psall", [128, 7 * 512], F32))
        banks = [psall[:, i * 512:(i + 1) * 512] for i in range(7)]
        ptb = es.enter_context(nc.psum_tensor("ptb", [128, 1024], BF16))

        block = es.enter_context(nc.Block())
        tr.block = block

        dumps = {}

        def dump(name, ap, shape, keys, ps_):
            if not DEBUG or ps_ != 0:
                return
            dd_ = nc.dram_tensor("dbg_" + name, list(shape), F32, kind="ExternalOutput").ap()
            dumps[name] = dd_
            tr.dma("gpsimd", "dbgd", keys, [], lambda e: e.dma_start(out=dd_, in_=ap))

        def act_copy(out, in_):
            return lambda e: e.activation(out=out, in_=in_, func=AF.Copy)

        tr.dma("gpsimd", "c", [], ["ident"], lambda e: e.dma_start(out=ident[:], in_=ident_d[:, :]))
        tr.dma("sync", "c", [], ["permf"], lambda e: e.dma_start(out=permf[:], in_=perm_d[:, :]))
        tr.dma("gpsimd", "c", [], ["maskg"], lambda e: e.dma_start(out=maskg[:], in_=maskg_d[:, :]))
        tr.dma("sync", "c", [], ["gvec"], lambda e: e.dma_start(out=gvec[:], in_=gvec_d[:, :, :]))
        tr.dma("sync", "c", [], ["sinks"], lambda e: e.dma_start(out=sinks[:], in_=sinks_d[:, :]))
        tr.dma("sync", "c", [], ["pscale"], lambda e: e.dma_start(out=pscale[:], in_=pscale_d[:, :]))
        tr.dma("sync", "c", [], ["ropec"], lambda e: e.dma_start(out=ropec[:], in_=ropec_d[:, :]))
        tr.dma("gpsimd", "c", [], ["skT"],
               lambda e: e.dma_start(out=skT[:], in_=skT_d.rearrange("a k n -> k a n")))
        tr.op("vector", [], ["onesf"], lambda e: e.memset(onesf[:], 1.0))
        tr.op("vector", [], ["epsT"], lambda e: e.memset(epsT[:], EPS))

        def rmsnorm(stack_tag, src, srckey, n, gi, dst, dstkey, bank_a, sq, rs, col0=0):
            pa = banks[bank_a]
            for c in range(NCH):
                sqc = sq[c % 2]
                tr.op("scalar", [(srckey, c)], [("sq", id(sqc))],
                      lambda e, c=c, sqc=sqc: e.activation(out=sqc[:, 0:n], in_=src[:, c, 0:n], func=AF.Square))
                tr.op("tensor", [("sq", id(sqc)), "onesf"], [("bank", bank_a)],
                      lambda e, c=c, sqc=sqc: e.matmul(pa[:, 0:n], lhsT=onesf[:], rhs=sqc[:, 0:n],
                                                       start=(c == 0), stop=(c == NCH - 1)))
            tr.op("scalar", [("bank", bank_a), "epsT"], [("rs", id(rs))],
                  lambda e: e.activation(out=rs[:, 0:n], in_=pa[:, 0:n], func=AF.Sqrt, bias=epsT[:], scale=1.0 / D))
            tr.op("vector", [("rs", id(rs))], [("rs", id(rs))],
                  lambda e: e.reciprocal(out=rs[:, 0:n], in_=rs[:, 0:n]))
            for c in range(NCH):
                tr.op("vector", [(srckey, c), ("rs", id(rs)), "gvec"], [(dstkey, c)],
                      lambda e, c=c: e.scalar_tensor_tensor(out=dst[:, c, col0:col0 + n], in0=src[:, c, 0:n],
                                                            scalar=gvec[:, gi, c:c + 1], in1=rs[:, 0:n],
                                                            op0=OP.mult, op1=OP.mult))

        wslot_ctr = [0]

        def load_w(wbuf, fn_dma_list):
            s = wslot_ctr[0] % len(wbuf)
            wslot_ctr[0] += 1
            for fn in fn_dma_list:
                tr.dma("gpsimd", f"w{s}", [], [("wbuf", s)], lambda e, fn=fn, s=s: fn(e, wbuf[s]))
            return s

        for ps_ in range(NPASS):
            with ExitStack() as p1:
                wbuf = [sb(p1, f"wbuf{i}", [128, NCH, 512], BF16) for i in range(3)]
                qT = sb(p1, "qT", [128, 8, T], BF16)
                kT = sb(p1, "kT", [128, 4, TH], BF16)
                Vsb = sb(p1, "Vsb", [128, 5, 256], BF16)
                uT = sb(p1, "uT", [128, 8, TH], F32)
                icnt = sb(p1, "icnt", [128, 4, 16], F32)
                wpool = sb(p1, "wpool", [128, 4, 2, 256], BF16)

                tr.dma("gpsimd", "c2", [], ["maskp"], lambda e: e.dma_start(out=maskp[:], in_=maskp_d[ps_, :, :]))
                tr.dma("sync", "c2", [], ["icnt"], lambda e: e.dma_start(out=icnt[:], in_=icnt_d[ps_, :, :, :]))
                tr.dma("gpsimd", "c2", [], ["wpool"],
                       lambda e: e.dma_start(out=wpool[:], in_=wpool_d.rearrange("g (cc p) n -> p g cc n", p=128)))
                for q4 in range(4):
                    tr.dma("sync", "x", [], [("hT", c) for c in range(4 * q4, 4 * q4 + 4)],
                           lambda e, q4=q4: e.dma_start(
                               out=hT[:, 4 * q4:4 * q4 + 4, :],
                               in_=xT_d[ps_, 512 * q4:512 * q4 + 512, HALO:TH].rearrange("(c p) t -> p c t", p=128)))

                with ExitStack() as a1:
                    xh = xn[:].rearrange("p c t -> p (c t)").bitcast(F32)[:, 0:NCH * HALO].rearrange(
                        "p (c t) -> p c t", c=NCH)
                    hn = s_all[:].rearrange("p a b c d -> p (a b c d)").bitcast(BF16)[:, 0:NCH * TH].rearrange(
                        "p (c t) -> p c t", c=NCH)
                    sq = [sb(a1, f"sq{i}", [128, T], F32) for i in range(2)]
                    rs = sb(a1, "rs", [128, T], F32)
                    rs2 = sb(a1, "rs2", [128, HALO], F32)
                    qraw = [sb(a1, f"qraw{i}", [128, TH], F32) for i in range(2)]
                    rt1 = [sb(a1, f"rt1_{i}", [128, TH], F32) for i in range(2)]
                    rt2 = [sb(a1, f"rt2_{i}", [128, TH], F32) for i in range(2)]
                    Ct = sb(a1, "Ct", [128, TH], F32)
                    St = sb(a1, "St", [128, TH], F32)
                    posi = qraw[1][:].bitcast(I32)
                    K_POSI = [("qraw", 1, 0), ("qraw", 1, 1)]
                    ang, ra, rb, ki = rt1[0], rt1[1], rt2[0], posi
                    K_ANG, K_RA = ("rt1", 0), ("rt1", 1)
                    K_RB = [("rt2", 0, 0), ("rt2", 0, 1)]

                    tr.dma("sync", "x", [], [("xh", c) for c in range(NCH)],
                           lambda e: e.dma_start(out=xh[:, :, :], in_=xT_d[ps_, :, 0:HALO].rearrange("(c p) t -> p c t", p=128)))
                    tr.dma("sync", "c2", [], K_POSI, lambda e: e.dma_start(out=posi, in_=pos_d[ps_, :, :]))

                    tr.op("vector", K_POSI, [K_ANG], lambda e: e.tensor_copy(out=ang[:], in_=posi))
                    tr.op("vector", [K_ANG, "ropec"], [K_ANG],
                          lambda e: e.tensor_scalar(out=ang[:], in0=ang[:], scalar1=ropec[:, 0:1], scalar2=None, op0=OP.mult))

                    def sin_of(shift, dst, sgn):
                        tr.op("vector", [K_ANG], [K_RA],
                              lambda e: e.tensor_scalar(out=ra[:], in0=ang[:], scalar1=1.0 / TWO_PI,
                                                        scalar2=shift / TWO_PI, op0=OP.mult, op1=OP.add))
                        tr.op("vector", [K_RA], K_POSI, lambda e: e.tensor_copy(out=ki, in_=ra[:]))
                        tr.op("vector", K_POSI, [K_RA], lambda e: e.tensor_copy(out=ra[:], in_=ki))
                        tr.op("vector", [K_RA], [K_RA],
                              lambda e: e.tensor_scalar(out=ra[:], in0=ra[:], scalar1=-TWO_PI, scalar2=shift,
                                                        op0=OP.mult, op1=OP.add))
                        tr.op("vector", [K_RA, K_ANG], K_RB,
                              lambda e: e.tensor_tensor(out=rb[:], in0=ra[:], in1=ang[:], op=OP.add))
                        tr.op("vector", K_RB, [K_RA],
                              lambda e: e.tensor_scalar(out=ra[:], in0=rb[:], scalar1=math.pi, scalar2=-TWO_PI,
                                                        op0=OP.is_gt, op1=OP.mult))
                        tr.op("vector", [K_RA] + K_RB, K_RB,
                              lambda e: e.tensor_tensor(out=rb[:], in0=rb[:], in1=ra[:], op=OP.add))
                        tr.op("vector", K_RB, [K_RA],
                              lambda e: e.tensor_scalar(out=ra[:], in0=rb[:], scalar1=-math.pi, scalar2=TWO_PI,
                                                        op0=OP.is_lt, op1=OP.mult))
                        tr.op("vector", [K_RA] + K_RB, K_RB,
                              lambda e: e.tensor_tensor(out=rb[:], in0=rb[:], in1=ra[:], op=OP.add))
                        tr.op("vector", K_RB, K_RB,
                              lambda e: e.tensor_scalar(out=rb[:], in0=rb[:], scalar1=3.1415925, scalar2=-3.1415925,
                                                        op0=OP.min, op1=OP.max))
                        tr.op("scalar", K_RB, [dst[1]], lambda e: e.activation(out=dst[0][:], in_=rb[:], func=AF.Sin))
                        if sgn:
                            tr.op("vector", [dst[1], "ropec"], [dst[1]],
                                  lambda e: e.tensor_scalar(out=dst[0][:], in0=dst[0][:], scalar1=ropec[:, 1:2],
                                                            scalar2=None, op0=OP.mult))

                    sin_of(0.0, (St, "St"), True)
                    sin_of(math.pi / 2, (Ct, "Ct"), False)

                    rmsnorm("a", hT, "hT", T, 0, hn, "hn_o", 0, sq, rs, col0=HALO)
                    rmsnorm("b", xh, "xh", HALO, 0, hn, "hn_h", 1, sq, rs2, col0=0)
                    hn_keys = [("hn_o", c) for c in range(NCH)] + [("hn_h", c) for c in range(NCH)]

                    sv = load_w(wbuf, [lambda e, wb: e.dma_start(
                        out=wb[:, 0:NCH, 0:256], in_=wv_d.rearrange("(c p) n -> p c n", p=128))])
                    nxt = load_w(wbuf, [lambda e, wb: e.dma_start(
                        out=wb[:, 0:NCH, :], in_=wqku_d[:, 0:512].rearrange("(c p) n -> p c n", p=128))])
                    for tt in range(5):
                        bk = 2 + tt % 2
                        tr.op("tensor", hn_keys + [("wbuf", sv)], [("bank", bk)],
                              [lambda e, c=c, tt=tt, bk=bk: e.matmul(banks[bk][:, 0:256],
                                                                      lhsT=hn[:, c, tt * 128:(tt + 1) * 128],
                                                                      rhs=wbuf[sv][:, c, 0:256],
                                                                      start=(c == 0), stop=(c == NCH - 1))
                               for c in range(NCH)])
                        tr.op("scalar", [("bank", bk)], [("Vsb", tt)],
                              act_copy(Vsb[:, tt, :], banks[bk][:, 0:256]))

                    ev = 0
                    for cg in range(5):
                        cur = nxt
                        if cg < 4:
                            nxt = load_w(wbuf, [lambda e, wb, cg=cg: e.dma_start(
                                out=wb[:, 0:NCH, :],
                                in_=wqku_d[:, 512 * (cg + 1):512 * (cg + 2)].rearrange("(c p) n -> p c n", p=128))])
                        for oc in range(4):
                            kind = "q" if cg < 2 else ("k" if cg == 2 else "u")
                            ba, bb = (2, 3) if ev % 2 == 0 else (4, 5)
                            ev += 1
                            tr.op("tensor", hn_keys + [("wbuf", cur)], [("bank", ba)],
                                  [lambda e, c=c, oc=oc, ba=ba, cur=cur: e.matmul(
                                      banks[ba][:, :], lhsT=wbuf[cur][:, c, oc * 128:(oc + 1) * 128],
                                      rhs=hn[:, c, HALO:TH], start=(c == 0), stop=(c == NCH - 1))
                                   for c in range(NCH)])
                            if kind != "q":
                                tr.op("tensor", hn_keys + [("wbuf", cur)], [("bank", bb)],
                                      [lambda e, c=c, oc=oc, bb=bb, cur=cur: e.matmul(
                                          banks[bb][:, 0:HALO], lhsT=wbuf[cur][:, c, oc * 128:(oc + 1) * 128],
                                          rhs=hn[:, c, 0:HALO], start=(c == 0), stop=(c == NCH - 1))
                                       for c in range(NCH)])
                            if kind == "u":
                                ch = (cg - 3) * 4 + oc
                                tr.op("scalar", [("bank", ba)], [("uT", ch, 1)],
                                      act_copy(uT[:, ch, HALO:TH], banks[ba][:, :]))
                                tr.op("scalar", [("bank", bb)], [("uT", ch, 0)],
                                      act_copy(uT[:, ch, 0:HALO], banks[bb][:, 0:HALO]))
                                continue
                            qi = ev % 2
                            qr = qraw[qi]
                            lo = HALO if kind == "q" else 0
                            tr.op("scalar", [("bank", ba)], [("qraw", qi, 1)], act_copy(qr[:, HALO:TH], banks[ba][:, :]))
                            if kind == "k":
                                tr.op("scalar", [("bank", bb)], [("qraw", qi, 0)],
                                      act_copy(qr[:, 0:HALO], banks[bb][:, 0:HALO]))
                            tr.op("tensor", [("qraw", qi, 1), "permf"], [("bank", ba)],
                                  lambda e, qr=qr, ba=ba: e.matmul(banks[ba][:, :], lhsT=permf[:], rhs=qr[:, HALO:TH],
                                                                   start=True, stop=True))
                            if kind == "k":
                                tr.op("tensor", [("qraw", qi, 0), "permf"], [("bank", bb)],
                                      lambda e, qr=qr, bb=bb: e.matmul(banks[bb][:, 0:HALO], lhsT=permf[:],
                                                                       rhs=qr[:, 0:HALO], start=True, stop=True))
                            t1 = rt1[qi]
                            t2 = rt2[qi]
                            tr.op("gpsimd", [("qraw", qi, 1), ("qraw", qi, 0), "Ct"], [("rt1", qi)],
                                  lambda e, qr=qr, t1=t1, lo=lo: e.tensor_tensor(out=t1[:, lo:TH], in0=qr[:, lo:TH],
                                                                                in1=Ct[:, lo:TH], op=OP.mult))
                            tr.op("vector", [("bank", ba), "St"], [("rt2", qi, 1)],
                                  lambda e, t2=t2, ba=ba: e.tensor_tensor(out=t2[:, HALO:TH], in0=banks[ba][:, :],
                                                                          in1=St[:, HALO:TH], op=OP.mult))
                            if kind == "k":
                                tr.op("vector", [("bank", bb), "St"], [("rt2", qi, 0)],
                                      lambda e, t2=t2, bb=bb: e.tensor_tensor(out=t2[:, 0:HALO], in0=banks[bb][:, 0:HALO],
                                                                              in1=St[:, 0:HALO], op=OP.mult))
                            if kind == "q":
                                ch = cg * 4 + oc
                                tr.op("gpsimd", [("rt1", qi), ("rt2", qi, 1)], [("qT", ch)],
                                      lambda e, t1=t1, t2=t2, ch=ch: e.tensor_tensor(out=qT[:, ch, :], in0=t1[:, HALO:TH],
                                                                                    in1=t2[:, HALO:TH], op=OP.add))
                            else:
                                tr.op("gpsimd", [("rt1", qi), ("rt2", qi, 1), ("rt2", qi, 0)], [("kT", oc)],
                                      lambda e, t1=t1, t2=t2, oc=oc: e.tensor_tensor(out=kT[:, oc, :], in0=t1[:, :],
                                                                                    in1=t2[:, :], op=OP.add))
                    dump("hn", hn, [128, NCH, TH], hn_keys, ps_)
                    dump("qT", qT[:], [128, 8, T], [("qT", c) for c in range(8)], ps_)
                    dump("kT", kT[:], [128, 4, TH], [("kT", c) for c in range(4)], ps_)
                    dump("Vsb", Vsb[:], [128, 5, 256], [("Vsb", c) for c in range(5)], ps_)
                    dump("uT", uT[:], [128, 8, TH], [("uT", c, i) for c in range(8) for i in range(2)], ps_)
                    dump("Ct", Ct[:], [128, TH], ["Ct"], ps_)
                    dump("St", St[:], [128, TH], ["St"], ps_)
                    tr.barrier()

                with ExitStack() as a2:
                    Pf = [sb(a2, f"Pf{i}", [128, 4, 256], F32) for i in range(2)]
                    Pn = [sb(a2, f"Pn{i}", [128, 4, 256], BF16) for i in range(2)]
                    PTs = [sb(a2, f"PTs{i}", [128, 1024], BF16) for i in range(2)]
                    sm = [sb(a2, f"sm{i}", [128, 8, 4], F32) for i in range(2)]
                    nsinks = sb(a2, "nsinks", [128, 16], F32)
                    attnT = s_all[:].rearrange("p a b c d -> p (a b c d)").bitcast(BF16)[:, 0:8 * T].rearrange(
                        "p (h t) -> p h t", h=8)
                    pooled = xn[:, 0:8, :]
                    mixp = xn[:, 8:16, :]
                    pa_ = sb(a2, "pa_", [128, TH], F32)
                    pb_ = sb(a2, "pb_", [128, TH], F32)

                    def attn_front(it):
                        qb, g = it // 4, it % 4
                        msk, mkey = (maskp, "maskp") if qb == 0 else (maskg, "maskg")
                        pi = it % 2
                        b0, b1 = (0, 1) if pi == 0 else (2, 3)
                        smt = sm[pi]
                        fns = []
                        for hh in range(4):
                            hd = 4 * g + hh
                            bp = (hd % 2) * 64
                            bk = b0 if hh < 2 else b1
                            co = (hh % 2) * 256
                            fns.append(lambda e, hd=hd, bp=bp, bk=bk, co=co, qb=qb, g=g: e.matmul(
                                banks[bk][:, co:co + 256], lhsT=qT[bp:bp + 64, hd // 2, qb * 128:(qb + 1) * 128],
                                rhs=kT[bp:bp + 64, g, qb * 128:qb * 128 + 256], start=True, stop=False))
                            fns.append(lambda e, bk=bk, co=co, msk=msk: e.matmul(
                                banks[bk][:, co:co + 256], lhsT=ident[:], rhs=msk[:], start=False, stop=True))
                        tr.op("tensor", [("qT", c) for c in (2 * g, 2 * g + 1)] + [("kT", g), "ident", mkey],
                              [("bank", b0), ("bank", b1)], fns)
                        for half, bk in ((0, b0), (1, b1)):
                            tr.op("vector", [("bank", bk)], [("sm", pi, "mx", half)],
                                  lambda e, bk=bk, half=half, smt=smt: e.tensor_reduce(
                                      out=smt[:, 0, 2 * half:2 * half + 2],
                                      in_=banks[bk][:, :].rearrange("p (h k) -> p h k", h=2), axis=AX.X, op=OP.max))
                        tr.op("vector", [("sm", pi, "mx", 0), ("sm", pi, "mx", 1), "sinks"], [("sm", pi, "m2")],
                              lambda e, smt=smt, g=g: e.scalar_tensor_tensor(
                                  out=smt[:, 2, :], in0=smt[:, 0, :], scalar=-SCALE, in1=nsinks[:, 4 * g:4 * g + 4],
                                  op0=OP.mult, op1=OP.min))
                        tr.op("vector", [("sm", pi, "m2"), "sinks"], [("sm", pi, "m")],
                              lambda e, smt=smt, g=g: e.tensor_tensor(out=smt[:, 3, :], in0=smt[:, 2, :],
                                                                      in1=sinks[:, 4 * g:4 * g + 4], op=OP.add))
                        pf = Pf[pi]
                        fns = []
                        for hh in range(4):
                            bk = b0 if hh < 2 else b1
                            co = (hh % 2) * 256
                            fns.append(lambda e, hh=hh, bk=bk, co=co, pf=pf, smt=smt: e.activation(
                                out=pf[:, hh, :], in_=banks[bk][:, co:co + 256], func=AF.Exp,
                                bias=smt[:, 2, hh:hh + 1], scale=SCALE, accum_out=smt[:, 4, hh:hh + 1]))
                        fns.append(lambda e, smt=smt: e.activation(out=smt[:, 5, :], in_=smt[:, 3, :], func=AF.Exp))
                        tr.op("scalar", [("bank", b0), ("bank", b1), ("sm", pi, "m"), ("sm", pi, "m2")],
                              [("Pf", pi), ("sm", pi, "z")], fns)

                    def attn_back(it):
                        qb, g = it // 4, it % 4
                        pi = it % 2
                        bo = 4 + pi
                        smt = sm[pi]
                        pf = Pf[pi]
                        pn = Pn[pi]
                        tr.op("vector", [("sm", pi, "z")], [("sm", pi, "zz")],
                              lambda e, smt=smt: e.tensor_tensor(out=smt[:, 6, :], in0=smt[:, 4, :], in1=smt[:, 5, :],
                                                                 op=OP.add))
                        tr.op("vector", [("sm", pi, "zz")], [("sm", pi, "rz")],
                              lambda e, smt=smt: e.reciprocal(out=smt[:, 7, :], in_=smt[:, 6, :]))
                        tr.op("vector", [("Pf", pi), ("sm", pi, "rz")], [("Pn", pi)],
                              lambda e, smt=smt, pf=pf, pn=pn: e.tensor_tensor(
                                  out=pn[:], in0=pf[:], in1=smt[:, 7, :].unsqueeze(2).to_broadcast([128, 4, 256]),
                                  op=OP.mult))
                        fns = []
                        for kb in range(2):
                            for hh in range(4):
                                o = (kb * 4 + hh) * 128
                                fns.append(lambda e, kb=kb, hh=hh, o=o, pn=pn: e.transpose(
                                    out=ptb[:, o:o + 128], in_=pn[:, hh, kb * 128:(kb + 1) * 128], identity=ident[:]))
                        tr.op("tensor", [("Pn", pi), "ident"], ["ptb"], fns)
                        pts = PTs[pi]
                        tr.op("scalar", ["ptb"], [("PTs", pi)], act_copy(pts[:], ptb[:]))
                        tr.op("tensor", [("PTs", pi), ("Vsb", qb), ("Vsb", qb + 1)], [("bank", bo)],
                              [lambda e, kb=kb, par=par, pts=pts, bo=bo, qb=qb, g=g: e.matmul(
                                  banks[bo][par * 64:(par + 1) * 64, 0:256], lhsT=Vsb[:, qb + kb, g * 64:(g + 1) * 64],
                                  rhs=pts[:, kb * 512:(kb + 1) * 512].rearrange("p (a b q) -> p a b q", a=2, b=2)[:, :, par, :],
                                  start=(kb == 0), stop=(kb == 1))
                               for par in range(2) for kb in range(2)])
                        tr.op("vector", [("bank", bo)], [("attnT", 2 * g + a, qb) for a in range(2)],
                              lambda e, bo=bo, qb=qb, g=g: e.tensor_copy(
                                  out=attnT[:, 2 * g:2 * g + 2, qb * 128:(qb + 1) * 128],
                                  in_=banks[bo][:, 0:256].rearrange("p (a q) -> p a q", a=2)))

                    tr.op("vector", ["sinks"], ["nsinks"],
                          lambda e: e.tensor_scalar(out=nsinks[:], in0=sinks[:], scalar1=-1.0, scalar2=None, op0=OP.mult))
                    attn_front(0)
                    for it in range(16):
                        if it + 1 < 16:
                            attn_front(it + 1)
                        attn_back(it)

                    for ch in range(8):
                        gi = ch // 2
                        w = 2 ** (gi + 1)
                        src = None
                        bufs = [pa_, pb_]
                        names = ["pa_", "pb_"]
                        cur_ap, cur_key = None, None
                        sft = 1
                        lvl = 0
                        while sft < w:
                            dst = bufs[lvl % 2]
                            dk = names[lvl % 2]
                            lo_ = HALO - 17 + 2 * sft
                            if lvl == 0:
                                tr.op("gpsimd", [("uT", ch, 0), ("uT", ch, 1)], [dk],
                                      lambda e, dst=dst, sft=sft, ch=ch, lo_=lo_: e.tensor_tensor(
                                          out=dst[:, lo_:TH], in0=uT[:, ch, lo_:TH], in1=uT[:, ch, lo_ - sft:TH - sft], op=OP.add))
                            else:
                                srcb = bufs[(lvl - 1) % 2]
                                sk_ = names[(lvl - 1) % 2]
                                tr.op("gpsimd", [sk_], [dk],
                                      lambda e, dst=dst, srcb=srcb, sft=sft, lo_=lo_: e.tensor_tensor(
                                          out=dst[:, lo_:TH], in0=srcb[:, lo_:TH], in1=srcb[:, lo_ - sft:TH - sft], op=OP.add))
                            cur_ap, cur_key = dst, dk
                            sft *= 2
                            lvl += 1
                        tr.op("gpsimd", [cur_key, "icnt"], [cur_key],
                              lambda e, cur_ap=cur_ap, gi=gi: e.tensor_tensor(
                                  out=cur_ap[:, HALO:HALO + 16], in0=cur_ap[:, HALO:HALO + 16], in1=icnt[:, gi, :], op=OP.mult))
                        tr.op("gpsimd", [cur_key], [cur_key],
                              lambda e, cur_ap=cur_ap, w=w: e.tensor_scalar(
                                  out=cur_ap[:, HALO + 16:TH], in0=cur_ap[:, HALO + 16:TH], scalar1=1.0 / w, scalar2=None,
                                  op0=OP.mult))
                        tr.op("gpsimd", [cur_key, ("uT", ch, 1)], [("pooled", ch)],
                              lambda e, cur_ap=cur_ap, ch=ch: e.tensor_tensor(
                                  out=pooled[:, ch, :], in0=cur_ap[:, HALO:TH], in1=uT[:, ch, HALO:TH], op=OP.subtract))
                    for ch in range(8):
                        gi, ec = ch // 2, ch % 2
                        bk = ch % 2
                        tr.op("tensor", [("pooled", 2 * gi), ("pooled", 2 * gi + 1), "wpool"], [("bank", bk)],
                              [lambda e, cc=cc, gi=gi, ec=ec, bk=bk: e.matmul(
                                  banks[bk][:, :], lhsT=wpool[:, gi, cc, ec * 128:(ec + 1) * 128],
                                  rhs=pooled[:, 2 * gi + cc, :], start=(cc == 0), stop=(cc == 1)) for cc in range(2)])
                        tr.op("scalar", [("bank", bk), "pscale"], [("mixp", ch)],
                              lambda e, ch=ch, bk=bk: e.activation(out=mixp[:, ch, :], in_=banks[bk][:, :], func=AF.Copy,
                                                                   scale=pscale[:, ch:ch + 1]))

                    dump("attnT", attnT, [128, 8, T], [("attnT", pr, qb) for pr in range(8) for qb in range(4)], ps_)
                    dump("pooled", pooled, [128, 8, T], [("pooled", c) for c in range(8)], ps_)
                    dump("mixp", mixp, [128, 8, T], [("mixp", c) for c in range(8)], ps_)

                    def ld_wout(cg):
                        return load_w(wbuf, [lambda e, wb, cg=cg: e.dma_start(
                            out=wb[:, 0:NCH, :],
                            in_=wout_d[:, 512 * cg:512 * cg + 512].rearrange("(c p) n -> p c n", p=128))])

                    nxt = ld_wout(0)
                    for cg in range(4):
                        cur = nxt
                        if cg < 3:
                            nxt = ld_wout(cg + 1)
                        for oc in range(4):
                            dc = cg * 4 + oc
                            bk = 2 + dc % 2
                            fns = [lambda e, pr=pr, oc=oc, bk=bk, cur=cur: e.matmul(
                                banks[bk][:, :], lhsT=wbuf[cur][:, pr, oc * 128:(oc + 1) * 128], rhs=attnT[:, pr, :],
                                start=(pr == 0), stop=False) for pr in range(8)]
                            fns += [lambda e, c=c, oc=oc, bk=bk, cur=cur: e.matmul(
                                banks[bk][:, :], lhsT=wbuf[cur][:, 8 + c, oc * 128:(oc + 1) * 128], rhs=mixp[:, c, :],
                                start=False, stop=(c == 7)) for c in range(8)]
                            tr.op("tensor", [("attnT", pr, qb) for pr in range(8) for qb in range(4)] +
                                  [("mixp", c) for c in range(8)] + [("wbuf", cur)], [("bank", bk)], fns)
                            tr.op("vector", [("bank", bk), ("hT", dc)], [("hT", dc)],
                                  lambda e, dc=dc, bk=bk: e.tensor_tensor(out=hT[:, dc, :], in0=banks[bk][:, :],
                                                                          in1=hT[:, dc, :], op=OP.add))
                    tr.barrier()
                if DEBUG:
                    tr.dma("sync", "dbg", [("hT", c) for c in range(NCH)], [],
                           lambda e: e.dma_start(out=dbg_d[ps_, 0].rearrange("(c p) t -> p c t", p=128), in_=hT[:]))

                with ExitStack() as p0:
                    qp = sb(p0, "qp", [128, NCH, T], BF16)
                    sq = [sb(p0, f"sqb{i}", [128, T], F32) for i in range(2)]
                    rs = sb(p0, "rsb", [128, T], F32)
                    v12 = sb(p0, "v12", [128, 4, 2, 16], F32)
                    tmp128 = uT[:, 4, 0:512].rearrange("p (h n) -> p h n", h=4)
                    cand = uT[:, 0:2, :].rearrange("p c t -> p (c t)")[:, 0:1024].rearrange("p (h a b) -> p h a b", h=4, a=16)
                    tmp256 = uT[:, 2:4, :].rearrange("p c t -> p (c t)")[:, 0:1024].rearrange("p (h n) -> p h n", h=4)
                    ctop = sb(p0, "ctop", [128, 4, 8, 16], F32)
                    dd = sb(p0, "dd", [128, 8, 16], F32)
                    zz = sb(p0, "zz", [128, 5, 8], F32)

                    rmsnorm("c", hT, "hT", T, 1, xn, "xn", 0, sq, rs)
                    xn_keys = [("xn", c) for c in range(NCH)]
                    nxt = load_w(wbuf, [lambda e, wb: e.dma_start(
                        out=wb[:, 0:NCH, :], in_=wquery_d[:, 0:512].rearrange("(c p) n -> p c n", p=128))])
                    def emit_topk_chains(pairs):
                        chains = []
                        for hl, (tt, h) in enumerate(pairs):
                            ops = []
                            for a in range(2):
                                ops.append(([("s_all", tt, h // 2)], [("v12", hl, a, 0)],
                                            lambda e, a=a, h=h, hl=hl, tt=tt: e.max(out=v12[:, hl, a, 0:8],
                                                                                  in_=s_all[:, tt, h, a, :])))
                                ops.append(([("s_all", tt, h // 2), ("v12", hl, a, 0)], [("tmp128", hl)],
                                            lambda e, a=a, h=h, hl=hl, tt=tt: e.match_replace(
                                                out=tmp128[:, hl, :], in_to_replace=v12[:, hl, a, 0:8],
                                                in_values=s_all[:, tt, h, a, :], imm_value=-1e30)))
                                ops.append(([("tmp128", hl)], [("v12", hl, a, 1)],
                                            lambda e, a=a, hl=hl: e.max(out=v12[:, hl, a, 8:16], in_=tmp128[:, hl, :])))
                            ops.append(([("v12", hl, a, i) for a in range(2) for i in range(2)], [("cand", hl)],
                                        lambda e, hl=hl: e.tensor_tensor(
                                            out=cand[:, hl, :, :],
                                            in0=v12[:, hl, 0, :].unsqueeze(2).to_broadcast([128, 16, 16]),
                                            in1=v12[:, hl, 1, :].unsqueeze(1).to_broadcast([128, 16, 16]), op=OP.add)))
                            ops.append(([("cand", hl)], [("ctop", tt, h, 0)],
                                        lambda e, h=h, hl=hl, tt=tt: e.max(out=ctop[:, tt, h, 0:8],
                                                                           in_=cand[:, hl, :, :].rearrange("p a b -> p (a b)"))))
                            ops.append(([("cand", hl), ("ctop", tt, h, 0)], [("tmp256", hl)],
                                        lambda e, h=h, hl=hl, tt=tt: e.match_replace(
                                            out=tmp256[:, hl, :], in_to_replace=ctop[:, tt, h, 0:8],
                                            in_values=cand[:, hl, :, :].rearrange("p a b -> p (a b)"), imm_value=-1e30)))
                            ops.append(([("tmp256", hl)], [("ctop", tt, h)],
                                        lambda e, h=h, hl=hl, tt=tt: e.max(out=ctop[:, tt, h, 8:16], in_=tmp256[:, hl, :])))
                            chains.append(ops)
                        for i in range(len(chains[0])):
                            for ops in chains:
                                r_, w_, f_ = ops[i]
                                tr.op("vector", r_, w_, f_)

                    for cg in range(4):
                        cur = nxt
                        if cg < 3:
                            nxt = load_w(wbuf, [lambda e, wb, cg=cg: e.dma_start(
                                out=wb[:, 0:NCH, :],
                                in_=wquery_d[:, 512 * (cg + 1):512 * (cg + 2)].rearrange("(c p) n -> p c n", p=128))])
                        for oc in range(4):
                            fc = cg * 4 + oc
                            bk = 1 + fc % 2
                            tr.op("tensor", xn_keys + [("wbuf", cur)], [("bank", bk)],
                                  [lambda e, c=c, oc=oc, bk=bk, cur=cur: e.matmul(
                                      banks[bk][:, :], lhsT=wbuf[cur][:, c, oc * 128:(oc + 1) * 128], rhs=xn[:, c, :],
                                      start=(c == 0), stop=(c == NCH - 1)) for c in range(NCH)])
                            tr.op("scalar", [("bank", bk)], [("qp", fc)], act_copy(qp[:, fc, :], banks[bk][:, :]))
                        b4 = cg
                        for tt in range(4):
                            bk = 3 + tt
                            tr.op("tensor", [("qp", fc) for fc in range(4 * b4, 4 * b4 + 4)] + ["skT"], [("bank", bk)],
                                  [lambda e, j=j, b4=b4, bk=bk, tt=tt: e.matmul(
                                      banks[bk][:, j * 128:(j + 1) * 128],
                                      lhsT=qp[:, 4 * b4 + j, tt * 128:(tt + 1) * 128], rhs=skT[:, j % 2, :],
                                      start=True, stop=True) for j in range(4)])
                            tr.op("scalar", [("bank", bk)], [("s_all", tt, b4)],
                                  lambda e, tt=tt, b4=b4, bk=bk: e.activation(
                                      out=s_all[:, tt, 2 * b4:2 * b4 + 2, :, :],
                                      in_=banks[bk][:, :].rearrange("p (h a n) -> p h a n", h=2, a=2), func=AF.Copy))
                        for tp in range(2):
                            emit_topk_chains([(2 * tp + t2, 2 * cg + hh) for t2 in range(2) for hh in range(2)])

                    ctop_all = ctop
                    for tt in range(4):
                        ctop = ctop_all[:, tt, :, :]
                        ck = [("ctop", tt, h) for h in range(8)] + [("ctop", tt, h, 0) for h in range(8)]
                        tr.op("vector", ck, ["dd"],
                              lambda e: e.tensor_tensor(out=dd[:], in0=ctop[:],
                                                        in1=ctop[:, :, 0:1].to_broadcast([128, 8, 16]), op=OP.subtract))
                        tr.op("scalar", ["dd"], ["dd"], lambda e: e.activation(out=dd[:], in_=dd[:], func=AF.Exp))
                        tr.op("vector", ["dd"], [("zz", 0)],
                              lambda e: e.tensor_reduce(out=zz[:, 0, :], in_=dd[:], axis=AX.X, op=OP.add))
                        tr.op("scalar", [("zz", 0)], [("zz", 1)],
                              lambda e: e.activation(out=zz[:, 1, :], in_=zz[:, 0, :], func=AF.Ln))
                        tr.op("vector", [("zz", 1)] + ck, [("zz", 2)],
                              lambda e: e.tensor_tensor(out=zz[:, 2, :], in0=zz[:, 1, :],
                                                        in1=ctop[:, :, 0:1].rearrange("p h o -> p (h o)"), op=OP.add))
                        tr.op("vector", ck, [("zz", 3)],
                              lambda e: e.tensor_scalar(out=zz[:, 3, :], in0=ctop[:, :, 15:16].rearrange("p h o -> p (h o)"),
                                                        scalar1=-MARGIN, scalar2=None, op0=OP.add))
                        tr.op("vector", [("zz", 3), ("zz", 2)], [("zz", 4)],
                              lambda e: e.tensor_tensor(out=zz[:, 4, :], in0=zz[:, 3, :], in1=zz[:, 2, :], op=OP.subtract))
                        tr.op("scalar", [("zz", 4)], [("tcm", tt)],
                              lambda e, tt=tt: e.activation(out=tcm[:, tt, :], in_=zz[:, 4, :], func=AF.Exp))
                        tr.op("vector", [("zz", 3)] + [("s_all", tt, b4) for b4 in range(4)],
                              [("s_all", tt, b4) for b4 in range(4)],
                              lambda e, tt=tt: e.tensor_tensor(
                                  out=s_all[:, tt, :, 0, :], in0=s_all[:, tt, :, 0, :],
                                  in1=zz[:, 3, :].unsqueeze(2).to_broadcast([128, 8, 128]), op=OP.subtract))
                        tr.op("vector", [("tcm", tt), "ident"], [("dg", tt, h) for h in range(8)],
                              lambda e, tt=tt: e.tensor_tensor(
                                  out=dg[:, tt, :, :], in0=ident[:].unsqueeze(1).to_broadcast([128, 8, 128]),
                                  in1=tcm[:, tt, :].unsqueeze(2).to_broadcast([128, 8, 128]), op=OP.mult))
                    tr.barrier()

            with ExitStack() as pp:
                ubuf = [sb(pp, f"ubuf{i}", [128, NCH, 512], BF16) for i in range(2)]
                vbuf = [sb(pp, f"vbuf{i}", [128, 4, D], BF16) for i in range(2)]
                gl = [sb(pp, f"gl{i}", [128, 4, T], BF16) for i in range(2)]
                GT = [sb(pp, f"GT{i}", [128, 4, T], BF16) for i in range(2)]
                NDB = 4
                Dc = [sb(pp, f"Dc{i}", [128, 2, 4, 128], BF16 if i < 3 else F32) for i in range(NDB)]
                Eb = [sb(pp, f"Eb{i}", [128, 2, 512], BF16) for i in range(NDB)]
                Wb = [sb(pp, f"Wb{i}", [128, 8, 512], BF16) for i in range(2)]
                xn_keys = [("xn", c) for c in range(NCH)]
                vbanks = [banks[3][:, :], banks[4][:, :], banks[5][:, :], banks[6][:, :]]
                vbkeys = [("bank", 3), ("bank", 4), ("bank", 5), ("bank", 6)]
                vall = psall[:, 3 * 512:7 * 512].rearrange("p (d t) -> p d t", d=4)

                def ld_u(g):
                    s = g % 2
                    if g == 0:
                        for c4 in range(4):
                            tr.dma("gpsimd", f"uq{c4}", [], [("ubuf", s, c4)], lambda e, c4=c4, s=s: e.dma_start(
                                out=ubuf[s][:, :, c4 * 128:(c4 + 1) * 128],
                                in_=UT_d[:, c4 * 128:(c4 + 1) * 128].rearrange("(c p) n -> p c n", p=128)))
                        return
                    tr.dma("gpsimd", f"u{s}", [], [("ubuf", s, c4) for c4 in range(4)], lambda e, g=g, s=s: e.dma_start(
                        out=ubuf[s][:, :, :], in_=UT_d[:, 512 * g:512 * g + 512].rearrange("(c p) n -> p c n", p=128)))

                def ld_v(g):
                    s = g % 2
                    for hv in range(2):
                        tr.dma("gpsimd", f"v{s}", [], [("vbuf", s, hv)], lambda e, g=g, s=s, hv=hv: e.dma_start(
                            out=vbuf[s][:, 2 * hv:2 * hv + 2, :],
                            in_=V_d[512 * g + 256 * hv:512 * g + 256 * hv + 256, :].rearrange("(c p) n -> p c n", p=128)))

                hbank = [banks[0][:, :], ptb[:].bitcast(F32)]
                hbkey = [("bank", 0), "ptb"]

                def emit_Hc_mm(g, c4):
                    s = g % 2
                    hb, hk = hbank[c4 % 2], hbkey[c4 % 2]
                    tr.op("tensor", xn_keys + [("ubuf", s, c4)], [hk],
                          [lambda e, c=c, c4=c4, hb=hb, s=s: e.matmul(
                              hb, lhsT=ubuf[s][:, c, c4 * 128:(c4 + 1) * 128], rhs=xn[:, c, :],
                              start=(c == 0), stop=(c == NCH - 1)) for c in range(NCH)])

                def emit_Hc_gelu(g, c4):
                    s = g % 2
                    hb, hk = hbank[c4 % 2], hbkey[c4 % 2]
                    tr.op("scalar", [hk], [("gl", s, c4)],
                          lambda e, c4=c4, hb=hb, s=s: e.activation(out=gl[s][:, c4, :], in_=hb, func=AF.Gelu))

                def emit_Hc(g, c4):
                    emit_Hc_mm(g, c4)
                    emit_Hc_gelu(g, c4)

                def emit_gate(g, tt):
                    k = 4 * g + tt
                    wb = Wb[k % 2]
                    wk = ("Wb", k % 2)

                    def parts(hp):
                        di = 4 * k + hp
                        return Dc[di % NDB], Eb[di % NDB], ("Dc", di % NDB), ("Eb", di % NDB), 2 * hp

                    def oplus(eng, hp):
                        dcb, ebb, dk, ek, h0 = parts(hp)
                        tr.op(eng, [("s_all", tt, hp)], [dk],
                              lambda e, dcb=dcb, tt=tt, h0=h0, g=g: e.tensor_tensor(
                                  out=dcb[:],
                                  in0=s_all[:, tt, h0:h0 + 2, 0, 4 * g:4 * g + 4].unsqueeze(3).to_broadcast([128, 2, 4, 128]),
                                  in1=s_all[:, tt, h0:h0 + 2, 1, :].unsqueeze(2).to_broadcast([128, 2, 4, 128]),
                                  op=OP.add))

                    def expo(hp):
                        dcb, ebb, dk, ek, h0 = parts(hp)
                        tr.op("scalar", [dk], [ek],
                              lambda e, dcb=dcb, ebb=ebb: e.activation(
                                  out=ebb[:], in_=dcb[:].rearrange("p h i j -> p h (i j)"), func=AF.Exp))

                    def stt(hp):
                        dcb, ebb, dk, ek, h0 = parts(hp)
                        tr.op("vector", [dk, ek], [(wk, hp)],
                              lambda e, dcb=dcb, ebb=ebb, wb=wb, h0=h0: e.scalar_tensor_tensor(
                                  out=wb[:, h0:h0 + 2, :], in0=dcb[:].rearrange("p h i j -> p h (i j)"),
                                  scalar=0.0, in1=ebb[:], op0=OP.is_ge, op1=OP.mult))

                    oplus("vector", 3)
                    oplus("gpsimd", 0)
                    expo(0)
                    expo(3)
                    stt(0)
                    stt(3)
                    for hp in (1, 2):
                        oplus("gpsimd", hp)
                        dcb, ebb, dk, ek, h0 = parts(hp)
                        tr.op("scalar", [dk], [dk],
                              lambda e, dcb=dcb: e.activation(out=dcb[:].rearrange("p h i j -> p h (i j)"),
                                                              in_=dcb[:].rearrange("p h i j -> p h (i j)"),
                                                              func=AF.Prelu, alpha=1.0e6))
                        tr.op("scalar", [dk], [(wk, hp)],
                              lambda e, dcb=dcb, wb=wb, h0=h0: e.activation(
                                  out=wb[:, h0:h0 + 2, :], in_=dcb[:].rearrange("p h i j -> p h (i j)"), func=AF.Exp))
                    bk = 1 + k % 2
                    tr.op("tensor", [(wk, hp) for hp in range(4)] + [("dg", tt, h) for h in range(8)], [("bank", bk)],
                          [lambda e, il=il, h=h, wb=wb, bk=bk, tt=tt: e.matmul(
                              banks[bk][:, il * 128:(il + 1) * 128], lhsT=wb[:, h, il * 128:(il + 1) * 128],
                              rhs=dg[:, tt, h, :], start=(h == 0), stop=(h == 7)) for il in range(4) for h in range(8)])

                def emit_G(g, tt):
                    k = 4 * g + tt
                    s = g % 2
                    bk = 1 + k % 2
                    tr.op("vector", [("bank", bk)] + [("gl", s, c4) for c4 in range(4)], [("GT", s, tt)],
                          lambda e, bk=bk, s=s, tt=tt: e.tensor_tensor(
                              out=GT[s][:, :, tt * 128:(tt + 1) * 128],
                              in0=banks[bk][:, :].rearrange("p (i q) -> p i q", i=4),
                              in1=gl[s][:, :, tt * 128:(tt + 1) * 128], op=OP.mult))

                def emit_Vmm(g, j):
                    s = g % 2
                    for d4 in range(4):
                        dc = 4 * j + d4
                        vb, vk = vbanks[d4], vbkeys[d4]
                        tr.op("tensor", [("GT", s, tt) for tt in range(4)] + [("vbuf", s, 0), ("vbuf", s, 1)], [vk],
                              [lambda e, c4=c4, dc=dc, vb=vb, s=s: e.matmul(
                                  vb, lhsT=vbuf[s][:, c4, dc * 128:(dc + 1) * 128], rhs=GT[s][:, c4, :],
                                  start=(c4 == 0), stop=(c4 == 3)) for c4 in range(4)])

                def emit_Vflush(g, j):
                    tr.op("vector", vbkeys + [("hT", 4 * j + d4) for d4 in range(4)], [("hT", 4 * j + d4) for d4 in range(4)],
                          lambda e, j=j: e.tensor_tensor(out=hT[:, 4 * j:4 * j + 4, :], in0=vall,
                                                         in1=hT[:, 4 * j:4 * j + 4, :], op=OP.add))

                ld_u(0)
                ld_u(1)
                ld_v(0)
                for c4 in range(4):
                    emit_Hc(0, c4)
                NSLOT = 4 * NEXP_GROUPS
                for k in range(NSLOT + 6):
                    g, tt = k // 4, k % 4
                    hc = None
                    if k < NSLOT:
                        if tt >= 1 and g + 1 < NEXP_GROUPS:
                            hc = (g + 1, tt - 1)
                        if tt == 0 and g >= 1:
                            hc = (g, 3)
                        if hc:
                            emit_Hc_mm(*hc)
                        emit_gate(g, tt)
                        if hc and hc[1] % 2 == 1:
                            emit_Hc_gelu(hc[0], hc[1] - 1)
                            emit_Hc_gelu(hc[0], hc[1])
                        if tt == 3 and g + 2 < NEXP_GROUPS:
                            ld_u(g + 2)
                        if tt == 2 and g >= 1:
                            ld_v(g)
                    if 1 <= k <= NSLOT:
                        emit_G((k - 1) // 4, (k - 1) % 4)
                    if 5 <= k < NSLOT + 5:
                        emit_Vflush((k - 5) // 4, (k - 5) % 4)
                    if 4 <= k < NSLOT + 4:
                        emit_Vmm(g - 1, tt)
                tr.barrier()
            if DEBUG:
                tr.dma("sync", "dbg", [("hT", c) for c in range(NCH)], [],
                       lambda e: e.dma_start(out=dbg_d[ps_, 1].rearrange("(c p) t -> p c t", p=128), in_=hT[:]))

            with ExitStack() as pg:
                wbuf = [sb(pg, f"wbufg{i}", [128, NCH, 512], BF16) for i in range(3)]
                wpj = sb(pg, "wpj", [128, 2, D], BF16)
                pTs = sb(pg, "pTs", [128, 2, T], BF16)
                sq = [sb(pg, f"sqg{i}", [128, T], F32) for i in range(2)]
                rs = sb(pg, "rsg", [128, T], F32)
                sg = [sb(pg, f"sg{i}", [128, T], F32) for i in range(2)]
                oT = sb(pg, "oT", [128, NCH, T], F32)

                tr.dma("gpsimd", "c3", [], ["wpj"],
                       lambda e: e.dma_start(out=wpj[:], in_=wproj_d.rearrange("(c p) n -> p c n", p=128)))
                tr.dma("gpsimd", "c3", [], ["pTs"],
                       lambda e: e.dma_start(out=pTs[:], in_=pT_d[ps_].rearrange("(c p) t -> p c t", p=128)))
                rmsnorm("d", hT, "hT", T, 2, xn, "xn", 0, sq, rs)
                xn_keys = [("xn", c) for c in range(NCH)]
                nxt = load_w(wbuf, [lambda e, wb: e.dma_start(
                    out=wb[:, 0:NCH, :], in_=wgate_d[:, 0:512].rearrange("(c p) n -> p c n", p=128))])
                for cg in range(4):
                    cur = nxt
                    if cg < 3:
                        nxt = load_w(wbuf, [lambda e, wb, cg=cg: e.dma_start(
                            out=wb[:, 0:NCH, :],
                            in_=wgate_d[:, 512 * (cg + 1):512 * (cg + 2)].rearrange("(c p) n -> p c n", p=128))])
                    for oc in range(4):
                        dc = cg * 4 + oc
                        bk = 1 + 2 * (dc % 2)
                        bk2 = bk + 1
                        tr.op("tensor", xn_keys + [("wbuf", cur)], [("bank", bk)],
                              [lambda e, c=c, oc=oc, bk=bk, cur=cur: e.matmul(
                                  banks[bk][:, :], lhsT=wbuf[cur][:, c, oc * 128:(oc + 1) * 128], rhs=xn[:, c, :],
                                  start=(c == 0), stop=(c == NCH - 1)) for c in range(NCH)])
                        tr.op("tensor", ["wpj", "pTs"], [("bank", bk2)],
                              [lambda e, c=c, dc=dc, bk2=bk2: e.matmul(
                                  banks[bk2][:, :], lhsT=wpj[:, c, dc * 128:(dc + 1) * 128], rhs=pTs[:, c, :],
                                  start=(c == 0), stop=(c == 1)) for c in range(2)])
                        sgi = sg[dc % 2]
                        tr.op("scalar", [("bank", bk)], [("sg", dc % 2)],
                              lambda e, sgi=sgi, bk=bk: e.activation(out=sgi[:], in_=banks[bk][:, :], func=AF.Sigmoid))
                        tr.op("vector", [("sg", dc % 2), ("bank", bk2)], [("sg", dc % 2)],
                              lambda e, sgi=sgi, bk2=bk2: e.tensor_tensor(out=sgi[:], in0=banks[bk2][:, :], in1=sgi[:],
                                                                          op=OP.mult))
                        tr.op("vector", [("sg", dc % 2), ("hT", dc)], [("hT", dc)],
                              lambda e, sgi=sgi, dc=dc: e.tensor_tensor(out=hT[:, dc, :], in0=hT[:, dc, :], in1=sgi[:],
                                                                        op=OP.add))
                if DEBUG:
                    tr.dma("sync", "dbg", [("hT", c) for c in range(NCH)], [],
                           lambda e: e.dma_start(out=dbg_d[ps_, 2].rearrange("(c p) t -> p c t", p=128), in_=hT[:]))
                rmsnorm("e", hT, "hT", T, 3, oT, "oT", 0, sq, rs)
                for q4 in range(4):
                    tr.dma("sync", "out", [("oT", c) for c in range(4 * q4, 4 * q4 + 4)], [],
                           lambda e, q4=q4: e.dma_start(
                               out=out_d[ps_, 512 * q4:512 * q4 + 512, :].rearrange("(c p) t -> p c t", p=128),
                               in_=oT[:, 4 * q4:4 * q4 + 4, :]))
                tr.barrier()

        tr.flush()
    return nc


_NC_CACHE = {}


def _consts():
    ident = np.eye(128, dtype=np.float32)
    perm = np.zeros((128, 128), np.float32)
    for p in range(128):
        r = p % 64
        if r < 8:
            perm[p + 8, p] = 1.0
        elif r < 16:
            perm[p - 8, p] = 1.0
        else:
            perm[p, p] = 1.0
    inv_freq = 1.0 / (500000.0 ** (np.arange(0, 16, 2, dtype=np.float32) / np.float32(16)))
    ropec = np.zeros((128, 2), np.float32)
    for p in range(128):
        r = p % 64
        if r < 16:
            ropec[p, 0] = inv_freq[r % 8]
            ropec[p, 1] = -1.0 if r < 8 else 1.0
    q = np.arange(128)[:, None]
    kj = np.arange(256)[None, :]
    rel = q + 128 - kj
    band = (rel >= 0) & (rel < 128)
    maskg = np.where(band, 0.0, -1e30).astype(np.float32)
    maskf = np.where(band & (kj >= 128), 0.0, -1e30).astype(np.float32)
    return ident, perm, ropec, maskg, maskf


def kernel(x, p, positions, g_mix, w_in, sinks, w_pool, pool_scale, w_out, g_ffn,
           w_query, sub_keys, expert_u, expert_v, g_ple, w_ple_gate, w_ple_proj, g_final):
    f = lambda a: np.ascontiguousarray(np.asarray(a), dtype=np.float32)
    x = f(x); p = f(p)
    positions = np.asarray(positions).astype(np.int32)
    w_in = f(w_in)[0]
    ident, perm, ropec, maskg, maskf = _consts()

    wq = w_in[:, 0:1024]
    wk = w_in[:, 1024:1280].reshape(D, 4, 64)
    kdup = np.concatenate([wk, wk], axis=2).reshape(D, 512)
    wu = w_in[:, 1536:2560]
    wqku = np.ascontiguousarray(np.concatenate([wq, kdup, wu], axis=1))
    wv = np.ascontiguousarray(w_in[:, 1280:1536])
    gvec = np.stack([f(g_mix)[0], f(g_ffn)[0], f(g_ple)[0], f(g_final)], axis=0)
    gvec = np.ascontiguousarray(gvec.reshape(4, NCH, 128).transpose(2, 0, 1))
    sinksb = np.ascontiguousarray(np.broadcast_to(f(sinks)[0][None, :], (128, 16)))
    pscale = np.ascontiguousarray(f(pool_scale)[0].reshape(8, 128).T)
    skT = np.ascontiguousarray(f(sub_keys)[0].transpose(0, 2, 1))
    UT = np.ascontiguousarray(f(expert_u)[0].T)
    Vx = f(expert_v)[0]

    shared = dict(maskg=maskg, wqku=wqku, wv=wv, wpool=f(w_pool)[0], pscale=pscale, gvec=gvec, sinksb=sinksb,
                  wout=f(w_out)[0], wquery=f(w_query)[0], skT=skT, UT=UT, Vx=Vx, wgate=f(w_ple_gate)[0],
                  wproj=f(w_ple_proj)[0], ident=ident, perm=perm, ropec=ropec)
    in_maps = []
    for c in range(NCORES):
        b = c // 4
        base = (c % 4) * 1024
        xT = np.zeros((NPASS, D, TH), np.float32)
        pT = np.zeros((NPASS, 256, T), np.float32)
        posb = np.zeros((NPASS, 128, TH), np.int32)
        maskp = np.zeros((NPASS, 128, 256), np.float32)
        icnt = np.zeros((NPASS, 128, 4, 16), np.float32)
        for ps_ in range(NPASS):
            st = base + ps_ * T
            xT[ps_, :, HALO:] = x[b, st:st + T, :].T
            posb[ps_, :, HALO:] = positions[b, st:st + T][None, :]
            if st > 0:
                xT[ps_, :, :HALO] = x[b, st - HALO:st, :].T
                posb[ps_, :, :HALO] = positions[b, st - HALO:st][None, :]
                maskp[ps_] = maskg
            else:
                maskp[ps_] = maskf
            pT[ps_] = p[0, b, st:st + T, :].T
            for gi, w in enumerate((2, 4, 8, 16)):
                tpos = st + np.arange(16)
                icnt[ps_, :, gi, :] = (1.0 / np.minimum(tpos + 1, w).astype(np.float32))[None, :]
        d = dict(shared)
        d.update(xT=xT, pT=pT, posb=posb, maskp=maskp, icnt=icnt)
        in_maps.append(d)

    if "nc" not in _NC_CACHE:
        _NC_CACHE["nc"] = build_program()
    nc = _NC_CACHE["nc"]
    res = run_bass_kernel_spmd(nc, in_maps, core_ids=list(range(NCORES)))
    out = np.zeros((2, 4096, D), np.float32)
    for c in range(NCORES):
        b = c // 4
        base = (c % 4) * 1024
        oT = res.results[c]["outT"]
        for ps_ in range(NPASS):
            st = base + ps_ * T
            out[b, st:st + T, :] = oT[ps_].T
    if DEBUG:
        kernel.dbg = [res.results[c]["dbg"] for c in range(NCORES)]
        kernel.dumps = [{k: v for k, v in res.results[c].items() if k.startswith("dbg_")} for c in range(NCORES)]
    return out
```
